# Optimizing a Trainium2 kernel written in Bass

```python
import math
import jax, jax.numpy as jnp
from jax import lax
import numpy as np

D_MODEL = 1024
BATCH = 2
SEQ = 16384
DEPTH = 2

N_META = 16
BLOCK_Q = 128
ROPE_THETA = 10000.0
RMS_EPS = 1e-6
DA_HEADS = 8
DA_HEAD_DIM = 64
DSA_HEADS = 16
DSA_HEAD_DIM = 64
IDX_HEADS = 8
IDX_DIM = 64
TOPK_MAX = 256
DSA_QKV = DSA_HEADS * DSA_HEAD_DIM
DSA_SPLITS = [DSA_QKV, 2 * DSA_QKV, 3 * DSA_QKV,
              3 * DSA_QKV + IDX_HEADS * IDX_DIM,
              3 * DSA_QKV + IDX_HEADS * IDX_DIM + IDX_DIM]
DSA_IN_WIDTH = DSA_SPLITS[-1] + IDX_HEADS
ROPE_DIM = 64
BIG_SCORE = 1e30
FFN_HIDDEN = 2816
CONV_WIDTH = 3

kernel_name = "hybrid_diffattn_dsa_convffn_meta"


def rms_norm(x, g):
    xf = x.astype(jnp.float32)
    y = xf * lax.rsqrt(jnp.mean(xf * xf, axis=-1, keepdims=True) + RMS_EPS)
    return (y * g.astype(jnp.float32)).astype(x.dtype)


def rope_tables(T, dim):
    inv = ROPE_THETA ** (-jnp.arange(0, dim, 2, dtype=jnp.float32) / dim)
    ang = jnp.arange(T, dtype=jnp.float32)[:, None] * inv[None, :]
    return jnp.cos(ang), jnp.sin(ang)


def apply_rope(x, cos, sin):
    shape = (1, cos.shape[0]) + (1,) * (x.ndim - 3) + (cos.shape[1],)
    c = cos.reshape(shape)
    s = sin.reshape(shape)
    x1, x2 = jnp.split(x.astype(jnp.float32), 2, axis=-1)
    return jnp.concatenate([x1 * c - x2 * s, x2 * c + x1 * s], axis=-1).astype(x.dtype)


def sweep_blocks(fn, T):
    out = lax.map(fn, jnp.arange(T // BLOCK_Q) * BLOCK_Q)
    out = jnp.moveaxis(out, 0, 1)
    return out.reshape((out.shape[0], T) + out.shape[3:])


def diff_attention(h, w_qkv, lq1, lk1, lq2, lk2, subln, w_o, cos, sin, lambda_init):
    B, T, _ = h.shape
    q, k, v = jnp.split(h @ w_qkv, 3, axis=-1)
    q = apply_rope(q.reshape(B, T, DA_HEADS, 2, DA_HEAD_DIM), cos, sin) * (DA_HEAD_DIM ** -0.5)
    k = apply_rope(k.reshape(B, T, DA_HEADS, 2, DA_HEAD_DIM), cos, sin)
    v = v.reshape(B, T, DA_HEADS, 2 * DA_HEAD_DIM)
    f32 = jnp.float32
    lam = (jnp.exp(jnp.sum(lq1.astype(f32) * lk1.astype(f32)))
           - jnp.exp(jnp.sum(lq2.astype(f32) * lk2.astype(f32))) + lambda_init)
    k_pos = jnp.arange(T)

    def block(start):
        q_pos = start + jnp.arange(BLOCK_Q)
        qb = lax.dynamic_slice_in_dim(q, start, BLOCK_Q, axis=1)
        s = jnp.einsum('bqhcd,bkhcd->bhcqk', qb, k).astype(f32)
        s = jnp.where(k_pos[None, :] <= q_pos[:, None], s, -jnp.inf)
        p = jax.nn.softmax(s, axis=-1)
        a = p[:, :, 0] - lam * p[:, :, 1]
        return jnp.einsum('bhqk,bkhe->bqhe', a.astype(v.dtype), v)

    o = sweep_blocks(block, T)
    o = rms_norm(o, subln) * (1.0 - lambda_init)
    return o.reshape(B, T, DA_HEADS * 2 * DA_HEAD_DIM) @ w_o


def dsa_attention(h, w_in, idx_k_norm, w_o, cos, sin, n_keys):
    B, T, _ = h.shape
    q, k, v, qi, ki, wi = jnp.split(h @ w_in, DSA_SPLITS, axis=-1)
    q = apply_rope(q.reshape(B, T, DSA_HEADS, DSA_HEAD_DIM), cos, sin) * (DSA_HEAD_DIM ** -0.5)
    k = apply_rope(k.reshape(B, T, DSA_HEADS, DSA_HEAD_DIM), cos, sin)
    v = v.reshape(B, T, DSA_HEADS, DSA_HEAD_DIM)
    qi = apply_rope(qi.reshape(B, T, IDX_HEADS, IDX_DIM), cos, sin)
    ki = apply_rope(rms_norm(ki, idx_k_norm), cos, sin)
    wi = wi * (IDX_HEADS ** -0.5)
    topk = min(TOPK_MAX, n_keys // 4)
    k_pos = jnp.arange(T)
    f32 = jnp.float32
    gather = jax.vmap(lambda arr, idx: arr[idx])

    def block(start):
        q_pos = start + jnp.arange(BLOCK_Q)
        qb = lax.dynamic_slice_in_dim(q, start, BLOCK_Q, axis=1)
        qib = lax.dynamic_slice_in_dim(qi, start, BLOCK_Q, axis=1)
        wb = lax.dynamic_slice_in_dim(wi, start, BLOCK_Q, axis=1)
        raw = jnp.einsum('bqhd,bkd->bqhk', qib, ki).astype(f32) * (IDX_DIM ** -0.5)
        score = jnp.einsum('bqhk,bqh->bqk', jax.nn.relu(raw), wb.astype(f32))
        causal = k_pos[None, :] <= q_pos[:, None]
        score = jnp.where(causal & (k_pos < N_META)[None, :], BIG_SCORE, score)
        score = jnp.where(causal, score, -jnp.inf)
        top_val, top_idx = lax.top_k(score, topk)
        valid = top_val > -jnp.inf
        ks = gather(k, top_idx)
        vs = gather(v, top_idx)
        s = jnp.einsum('bqhd,bqjhd->bhqj', qb, ks).astype(f32)
        s = jnp.where(valid[:, None], s, -jnp.inf)
        p = jax.nn.softmax(s, axis=-1)
        return jnp.einsum('bhqj,bqjhd->bqhd', p.astype(vs.dtype), vs)

    o = sweep_blocks(block, T)
    return o.reshape(B, T, DSA_QKV) @ w_o


def conv_ffn(h, w_up, conv_w, conv_b, w_down):
    u = h @ w_up
    T = u.shape[1]
    up = jnp.pad(u, ((0, 0), (CONV_WIDTH - 1, 0), (0, 0)))
    c = conv_b + sum(conv_w[j] * up[:, j:j + T] for j in range(CONV_WIDTH))
    g, val = jnp.split(c, 2, axis=-1)
    return (jax.nn.silu(g) * val) @ w_down


def setup_inputs(seed: int = 0) -> dict:
    key = jax.random.key(seed)
    ks = iter(jax.random.split(key, 32))
    n_a = (DEPTH + 1) // 2
    n_b = DEPTH // 2
    D, F = D_MODEL, FFN_HIDDEN

    def nrm(shape, scale):
        return jax.random.normal(next(ks), shape, jnp.float32) * scale

    def gain(shape):
        return 1.0 + nrm(shape, 0.02)

    return {
        "x": nrm((BATCH, SEQ, D), 1.0),
        "meta_tokens": nrm((N_META, D), 1.0),
        "da_norm": gain((n_a, D)),
        "da_w_qkv": nrm((n_a, D, 3 * D), D ** -0.5),
        "da_lambda_q1": nrm((n_a, DA_HEAD_DIM), 0.1),
        "da_lambda_k1": nrm((n_a, DA_HEAD_DIM), 0.1),
        "da_lambda_q2": nrm((n_a, DA_HEAD_DIM), 0.1),
        "da_lambda_k2": nrm((n_a, DA_HEAD_DIM), 0.1),
        "da_subln": gain((n_a, 2 * DA_HEAD_DIM)),
        "da_w_o": nrm((n_a, DA_HEADS * 2 * DA_HEAD_DIM, D), D ** -0.5),
        "dsa_norm": gain((n_b, D)),
        "dsa_w_in": nrm((n_b, D, DSA_IN_WIDTH), D ** -0.5),
        "dsa_idx_k_norm": gain((n_b, IDX_DIM)),
        "dsa_w_o": nrm((n_b, DSA_QKV, D), DSA_QKV ** -0.5),
        "ffn_norm": gain((DEPTH, D)),
        "ffn_w_up": nrm((DEPTH, D, 2 * F), D ** -0.5),
        "ffn_conv_w": nrm((DEPTH, CONV_WIDTH, 2 * F), CONV_WIDTH ** -0.5),
        "ffn_conv_b": nrm((DEPTH, 2 * F), 0.02),
        "ffn_w_down": nrm((DEPTH, F, D), F ** -0.5),
        "final_norm": gain((D,)),
    }


def reference(x, meta_tokens, da_norm, da_w_qkv, da_lambda_q1, da_lambda_k1,
              da_lambda_q2, da_lambda_k2, da_subln, da_w_o, dsa_norm, dsa_w_in,
              dsa_idx_k_norm, dsa_w_o, ffn_norm, ffn_w_up, ffn_conv_w, ffn_conv_b,
              ffn_w_down, final_norm):
    B, L, D = x.shape
    T = L + N_META
    T_pad = ((T + BLOCK_Q - 1) // BLOCK_Q) * BLOCK_Q
    meta = jnp.broadcast_to(meta_tokens[None].astype(x.dtype), (B, N_META, D))
    h = jnp.concatenate([meta, x], axis=1)
    h = jnp.pad(h, ((0, 0), (0, T_pad - T), (0, 0)))
    cos, sin = rope_tables(T_pad, ROPE_DIM)
    for i in range(DEPTH):
        j = i // 2
        if i % 2 == 0:
            lambda_init = 0.8 - 0.6 * math.exp(-0.3 * i)
            h = h + diff_attention(rms_norm(h, da_norm[j]), da_w_qkv[j],
                                   da_lambda_q1[j], da_lambda_k1[j],
                                   da_lambda_q2[j], da_lambda_k2[j],
                                   da_subln[j], da_w_o[j], cos, sin, lambda_init)
        else:
            h = h + dsa_attention(rms_norm(h, dsa_norm[j]), dsa_w_in[j],
                                  dsa_idx_k_norm[j], dsa_w_o[j], cos, sin, L)
        h = h + conv_ffn(rms_norm(h, ffn_norm[i]), ffn_w_up[i], ffn_conv_w[i],
                         ffn_conv_b[i], ffn_w_down[i])
    h = rms_norm(h, final_norm)
    return h[:, N_META:N_META + L]
```

```python
import math
from contextlib import ExitStack

import numpy as np
import ml_dtypes

import concourse.bass as bass
import concourse.mybir as mybir
from concourse.bass_utils import run_bass_kernel_spmd

F32 = mybir.dt.float32
BF16 = mybir.dt.bfloat16
AF = mybir.ActivationFunctionType
ALU = mybir.AluOpType
AX = mybir.AxisListType
NPBF = ml_dtypes.bfloat16

D = 1024
B = 2
L = 16384
NMETA = 16
T = 16512
NBLK = 129
NCORE = 8
CB = 33
CT = CB * 128
FF = 2816
NFC = 22
EPS = 1e-6
BIGM = 1.0e30
BIGC = 3.0e38
TOPK = 256
N_BISECT = 18


class Buf:
    def __init__(self, t, name=""):
        self.t = t
        self.name = name
        self.w = None
        self.r = {}
        self.dsem = None
        self.dcnt = 0

    def __getitem__(self, idx):
        return self.t[idx]


class Sched:
    def __init__(self, nc, es):
        self.nc = nc
        self.es = es
        self.eng = {"pe": nc.tensor, "dve": nc.vector, "act": nc.scalar, "pool": nc.gpsimd, "sp": nc.sync}
        self.sem = {k: es.enter_context(nc.semaphore("sem_" + k)) for k in self.eng}
        self.cnt = {k: 0 for k in self.eng}
        self.seen = {k: {} for k in self.eng}
        self.nbuf = 0
        self.out_tokens = []

    def sbuf(self, name, shape, dt):
        t = self.es.enter_context(self.nc.sbuf_tensor(name, list(shape), dt))
        return Buf(t, name)

    def psum(self, name, shape, dt):
        t = self.es.enter_context(self.nc.psum_tensor(name, list(shape), dt))
        return Buf(t, name)

    def _wait(self, e, deps):
        eng = self.eng[e]
        for d in deps:
            if d is None:
                continue
            sem, val, owner = d
            if owner == e and e == "pe":
                continue
            key = id(sem)
            if self.seen[e].get(key, 0) >= val:
                continue
            eng.wait_ge(sem, val)
            self.seen[e][key] = val

    def _deps(self, reads, writes):
        deps = []
        for b in reads:
            deps.append(b.w)
        for b in writes:
            deps.append(b.w)
            deps.extend(b.r.values())
        return deps

    def op(self, e, fn, reads=(), writes=()):
        self._wait(e, self._deps(reads, writes))
        ins = fn(self.eng[e])
        self.cnt[e] += 1
        ins.then_inc(self.sem[e], 1)
        tok = (self.sem[e], self.cnt[e], e)
        for b in reads:
            b.r[e] = tok
        for b in writes:
            b.w = tok
            b.r = {}
        return ins

    def multi(self, e, fns, reads=(), writes=()):
        self._wait(e, self._deps(reads, writes))
        ins = None
        for fn in fns:
            ins = fn(self.eng[e])
        self.cnt[e] += 1
        ins.then_inc(self.sem[e], 1)
        tok = (self.sem[e], self.cnt[e], e)
        for b in reads:
            b.r[e] = tok
        for b in writes:
            b.w = tok
            b.r = {}
        return ins

    def dma(self, q, out, in_, sb, load=True, extra_reads=(), **kw):
        if load:
            deps = self._deps(extra_reads, [sb])
        else:
            deps = self._deps([sb] + list(extra_reads), [])
        self._wait(q, deps)
        if sb.dsem is None:
            sb.dsem = self.es.enter_context(self.nc.semaphore("d_" + sb.name))
        ins = self.eng[q].dma_start(out=out, in_=in_, **kw)
        sb.dcnt += 16
        ins.then_inc(sb.dsem, 16)
        tok = (sb.dsem, sb.dcnt, "dma")
        if load:
            sb.w = tok
            sb.r = {}
        else:
            sb.r["dma"] = tok
            self.out_tokens.append(tok)
        return ins

    def finish(self):
        last = {}
        for sem, val, owner in self.out_tokens:
            k = id(sem)
            if k not in last or last[k][1] < val:
                last[k] = (sem, val, owner)
        self._wait("sp", list(last.values()))
        for e in ("pe", "dve", "act", "pool"):
            if self.cnt[e] > 0:
                self._wait("sp", [(self.sem[e], self.cnt[e], e)])


def make_ident(S, nc):
    identf = S.sbuf("identf", [128, 128], F32)
    ident = S.sbuf("ident", [128, 128], BF16)
    S.op("pool", lambda e: e.memset(identf[:], 0.0), writes=[identf])
    S.op("pool", lambda e: e.affine_select(out=identf[:], in_=identf[:], pattern=[[-1, 128]],
                                           compare_op=ALU.not_equal, fill=1.0, base=0,
                                           channel_multiplier=1), reads=[identf], writes=[identf])
    S.op("pool", lambda e: e.tensor_copy(out=ident[:], in_=identf[:]), reads=[identf], writes=[ident])
    return ident


def load_weight_bf16(S, name, w_ap, ncols):
    wb = S.sbuf(name, [128, 8, ncols], BF16)
    src = w_ap.rearrange("(k p) n -> p k n", p=128)
    for k in range(8):
        S.dma("pool", wb[:, k, :], src[:, k, :], wb, load=True)
    return wb


def load_bcast(S, name, vec_ap, n, q="sp"):
    t = S.sbuf(name, [128, n], F32)
    S.dma(q, t[:], vec_ap.partition_broadcast(128), t, load=True)
    return t


def emit_rmsnorm_T(S, x_buf, x_ap, g_bc, hn, stat, junk, ptr, ident, dst_buf, dst_ap, ncol=1024):
    nch = ncol // 128
    S.op("act", lambda e: e.activation(out=junk[:, 0:ncol], in_=x_ap, func=AF.Square, accum_out=stat[:, 0:1]),
         reads=[x_buf], writes=[junk, stat])
    S.op("act", lambda e: e.activation(out=stat[:, 1:2], in_=stat[:, 0:1], func=AF.Sqrt, scale=1.0 / ncol, bias=EPS),
         reads=[stat], writes=[stat])
    S.op("dve", lambda e: e.reciprocal(out=stat[:, 2:3], in_=stat[:, 1:2]), reads=[stat], writes=[stat])
    S.op("dve", lambda e: e.scalar_tensor_tensor(out=hn[:, 0:ncol], in0=x_ap, scalar=stat[:, 2:3], in1=g_bc[:, 0:ncol],
                                                 op0=ALU.mult, op1=ALU.mult),
         reads=[x_buf, stat, g_bc], writes=[hn])
    S.multi("pe", [(lambda e, k=k: e.transpose(ptr[:, k, :], hn[:, k * 128:(k + 1) * 128], ident[:])) for k in range(nch)],
            reads=[hn, ident], writes=[ptr])
    S.op("act", lambda e: e.copy(out=dst_ap, in_=ptr[:, 0:nch, :]), reads=[ptr], writes=[dst_buf])


def emit_rope(S, src_buf, src_ap3, cos_ap, sin_ap, tmps, out_buf, out_ap3, nh):
    t1, t2, t3, t4 = tmps
    if nh > 1:
        cb = cos_ap.unsqueeze(1).broadcast_to([128, nh, 32])
        sb = sin_ap.unsqueeze(1).broadcast_to([128, nh, 32])
        v = lambda t: t[:, 0:nh * 32].rearrange("p (h d) -> p h d", d=32)
    else:
        cb, sb = cos_ap, sin_ap
        v = lambda t: t[:, 0:32]
        src_ap3 = src_ap3
    x1 = src_ap3[:, :, 0:32] if nh > 1 else src_ap3[:, 0:32]
    x2 = src_ap3[:, :, 32:64] if nh > 1 else src_ap3[:, 32:64]
    o1 = out_ap3[:, :, 0:32] if nh > 1 else out_ap3[:, 0:32]
    o2 = out_ap3[:, :, 32:64] if nh > 1 else out_ap3[:, 32:64]
    S.op("dve", lambda e: e.tensor_tensor(out=v(t1), in0=x1, in1=cb, op=ALU.mult), reads=[src_buf], writes=[t1])
    S.op("dve", lambda e: e.tensor_tensor(out=v(t2), in0=x2, in1=sb, op=ALU.mult), reads=[src_buf], writes=[t2])
    S.op("dve", lambda e: e.tensor_tensor(out=v(t3), in0=x2, in1=cb, op=ALU.mult), reads=[src_buf], writes=[t3])
    S.op("dve", lambda e: e.tensor_tensor(out=v(t4), in0=x1, in1=sb, op=ALU.mult), reads=[src_buf], writes=[t4])
    S.op("pool", lambda e: e.tensor_tensor(out=o1, in0=v(t1), in1=v(t2), op=ALU.subtract), reads=[t1, t2], writes=[out_buf])
    S.op("pool", lambda e: e.tensor_tensor(out=o2, in0=v(t3), in1=v(t4), op=ALU.add), reads=[t3, t4, out_buf], writes=[out_buf])


def build_proj(kind, nblk=CB):
    nc = bass.Bass("TRN2", target_bir_lowering=False)
    ncols = 3072 if kind == "A" else 3656
    ct = nblk * 128
    h_in = nc.dram_tensor("h_in", [ct, D], F32, kind="ExternalInput").ap()
    g_in = nc.dram_tensor("g_in", [D], F32, kind="ExternalInput").ap()
    w_in = nc.dram_tensor("w_in", [D, ncols], F32, kind="ExternalInput").ap()
    cs_in = nc.dram_tensor("cs_in", [128, nblk, 4, 32], F32, kind="ExternalInput").ap()
    qT_o = nc.dram_tensor("qT_o", [8, 128, ct], BF16, kind="ExternalOutput").ap()
    kT_o = nc.dram_tensor("kT_o", [8, 128, ct], BF16, kind="ExternalOutput").ap()
    if kind == "A":
        v_o = nc.dram_tensor("v_o", [ct, 8, 129], BF16, kind="ExternalOutput").ap()
    else:
        v_o = nc.dram_tensor("v_o", [ct, 16, 65], BF16, kind="ExternalOutput").ap()
        idxg_in = nc.dram_tensor("idxg_in", [64], F32, kind="ExternalInput").ap()
        qiT_o = nc.dram_tensor("qiT_o", [4, 128, ct], BF16, kind="ExternalOutput").ap()
        kiT_o = nc.dram_tensor("kiT_o", [64, ct], BF16, kind="ExternalOutput").ap()
        wi_o = nc.dram_tensor("wi_o", [ct, 8], F32, kind="ExternalOutput").ap()
    with ExitStack() as es:
        S = Sched(nc, es)
        ident = make_ident(S, nc)
        g_bc = load_bcast(S, "g_bc", g_in, D)
        cs = S.sbuf("cs", [128, nblk, 4, 32], F32)
        S.dma("sp", cs[:], cs_in, cs, load=True)
        wb = load_weight_bf16(S, "wb", w_in, ncols)
        if kind == "B":
            idxg = load_bcast(S, "idxg", idxg_in, 64)
        hb = [S.sbuf("hb%d" % i, [128, D], F32) for i in range(2)]
        hn = [S.sbuf("hn%d" % i, [128, D], BF16) for i in range(2)]
        stat = [S.sbuf("stat%d" % i, [128, 8], F32) for i in range(2)]
        junk = S.sbuf("junk", [128, D], BF16)
        hnT = [S.sbuf("hnT%d" % i, [128, 8, 128], BF16) for i in range(2)]
        ptr = [S.psum("ptr%d" % i, [128, 8, 128], BF16) for i in range(1)]
        pA = S.psum("pA", [128, 1024], F32)
        pB = S.psum("pB", [128, 1024], F32)
        pC = S.psum("pC", [128, 1024], F32)
        if kind == "B":
            pD = S.psum("pD", [128, 512], F32)
        tq = [S.sbuf("tq%d" % i, [128, 512], F32) for i in range(4)]
        tk = [S.sbuf("tk%d" % i, [128, 512], F32) for i in range(4)]
        qr = [S.sbuf("qr%d" % i, [128, 1024], BF16) for i in range(2)]
        kr = [S.sbuf("kr%d" % i, [128, 1024], BF16) for i in range(2)]
        qTs = [S.sbuf("qTs%d" % i, [128, 8, 128], BF16) for i in range(2)]
        kTs = [S.sbuf("kTs%d" % i, [128, 8, 128], BF16) for i in range(2)]
        if kind == "A":
            vs = [S.sbuf("vs%d" % i, [128, 8, 129], BF16) for i in range(2)]
        else:
            vs = [S.sbuf("vs%d" % i, [128, 16, 65], BF16) for i in range(2)]
            tqi = [S.sbuf("tqi%d" % i, [128, 256], F32) for i in range(4)]
            qir = [S.sbuf("qir%d" % i, [128, 512], BF16) for i in range(2)]
            qiTs = [S.sbuf("qiTs%d" % i, [128, 4, 128], BF16) for i in range(2)]
            kin = S.sbuf("kin", [128, 64], F32)
            tki = [S.sbuf("tki%d" % i, [128, 32], F32) for i in range(4)]
            kir = [S.sbuf("kir%d" % i, [128, 64], BF16) for i in range(2)]
            kiTs = [S.sbuf("kiTs%d" % i, [64, 128], BF16) for i in range(2)]
            wis = [S.sbuf("wis%d" % i, [128, 8], F32) for i in range(2)]
            kstat = S.sbuf("kstat", [128, 8], F32)
            kjunk = S.sbuf("kjunk", [128, 64], F32)
        for v in vs:
            S.op("pool", lambda e, v=v: e.memset(v[:], 1.0), writes=[v])

        def proj(pbuf, col0, n, hT):
            fns = []
            for c0 in range(0, n, 512):
                cw = min(512, n - c0)
                for k in range(8):
                    fns.append(lambda e, c0=c0, cw=cw, k=k: e.matmul(
                        pbuf[:, c0:c0 + cw], lhsT=hT[:, k, :], rhs=wb[:, k, col0 + c0:col0 + c0 + cw],
                        start=(k == 0), stop=(k == 7)))
            S.multi("pe", fns, reads=[hT, wb], writes=[pbuf])

        for i in range(nblk):
            p = i % 2
            tsl = slice(i * 128, (i + 1) * 128)
            S.dma("sp", hb[p][:], h_in[tsl, :], hb[p], load=True)
            emit_rmsnorm_T(S, hb[p], hb[p][:], g_bc, hn[p], stat[p], junk, ptr[0], ident, hnT[p], hnT[p][:])
            proj(pA, 0, 1024, hnT[p])
            proj(pB, 1024, 1024, hnT[p])
            proj(pC, 2048, 1024, hnT[p])
            emit_rope(S, pA, pA[:].rearrange("p (h d) -> p h d", d=64), cs[:, i, 0, :], cs[:, i, 1, :], tq,
                      qr[p], qr[p][:].rearrange("p (h d) -> p h d", d=64), 16)
            emit_rope(S, pB, pB[:].rearrange("p (h d) -> p h d", d=64), cs[:, i, 2, :], cs[:, i, 3, :], tk,
                      kr[p], kr[p][:].rearrange("p (h d) -> p h d", d=64), 16)
            if kind == "A":
                S.op("act", lambda e: e.copy(out=vs[p][:, :, 0:128], in_=pC[:].rearrange("p (h d) -> p h d", d=128)),
                     reads=[pC], writes=[vs[p]])
            else:
                S.op("act", lambda e: e.copy(out=vs[p][:, :, 0:64], in_=pC[:].rearrange("p (h d) -> p h d", d=64)),
                     reads=[pC], writes=[vs[p]])
            S.dma("sp", v_o[tsl, :, :], vs[p][:], vs[p], load=False)
            for (src, stg, dst) in ((qr[p], qTs[p], qT_o), (kr[p], kTs[p], kT_o)):
                S.multi("pe", [(lambda e, k=k, src=src: e.transpose(ptr[0][:, k, :], src[:, k * 128:(k + 1) * 128], ident[:]))
                               for k in range(8)], reads=[src, ident], writes=[ptr[0]])
                S.op("act", lambda e, stg=stg: e.copy(out=stg[:], in_=ptr[0][:]), reads=[ptr[0]], writes=[stg])
                S.dma("sp", dst[:, :, tsl].rearrange("c p t -> p c t"), stg[:], stg, load=False)
            if kind == "B":
                proj(pD, 3072, 512, hnT[p])
                emit_rope(S, pD, pD[:].rearrange("p (h d) -> p h d", d=64), cs[:, i, 0, :], cs[:, i, 1, :], tqi,
                          qir[p], qir[p][:].rearrange("p (h d) -> p h d", d=64), 8)
                S.multi("pe", [(lambda e, k=k: e.transpose(ptr[0][:, k, :], qir[p][:, k * 128:(k + 1) * 128], ident[:]))
                               for k in range(4)], reads=[qir[p], ident], writes=[ptr[0]])
                S.op("act", lambda e: e.copy(out=qiTs[p][:], in_=ptr[0][:, 0:4, :]), reads=[ptr[0]], writes=[qiTs[p]])
                S.dma("sp", qiT_o[:, :, tsl].rearrange("c p t -> p c t"), qiTs[p][:], qiTs[p], load=False)
                proj(pD, 3584, 72, hnT[p])
                S.op("act", lambda e: e.activation(out=kjunk[:], in_=pD[:, 0:64], func=AF.Square, accum_out=kstat[:, 0:1]),
                     reads=[pD], writes=[kjunk, kstat])
                S.op("act", lambda e: e.activation(out=kstat[:, 1:2], in_=kstat[:, 0:1], func=AF.Sqrt, scale=1.0 / 64, bias=EPS),
                     reads=[kstat], writes=[kstat])
                S.op("dve", lambda e: e.reciprocal(out=kstat[:, 2:3], in_=kstat[:, 1:2]), reads=[kstat], writes=[kstat])
                S.op("dve", lambda e: e.scalar_tensor_tensor(out=kin[:], in0=pD[:, 0:64], scalar=kstat[:, 2:3], in1=idxg[:],
                                                             op0=ALU.mult, op1=ALU.mult),
                     reads=[pD, kstat, idxg], writes=[kin])
                S.op("dve", lambda e: e.tensor_scalar(out=wis[p][:], in0=pD[:, 64:72], scalar1=float(8 ** -0.5), scalar2=None,
                                                      op0=ALU.mult), reads=[pD], writes=[wis[p]])
                S.dma("sp", wi_o[tsl, :], wis[p][:], wis[p], load=False)
                emit_rope(S, kin, kin[:], cs[:, i, 2, :], cs[:, i, 3, :], tki, kir[p], kir[p][:], 1)
                S.op("pe", lambda e: e.transpose(ptr[0][0:64, 0, :], kir[p][:, 0:64], ident[:]),
                     reads=[kir[p], ident], writes=[ptr[0]])
                S.op("act", lambda e: e.copy(out=kiTs[p][:], in_=ptr[0][0:64, 0, :]), reads=[ptr[0]], writes=[kiTs[p]])
                S.dma("sp", kiT_o[:, tsl], kiTs[p][:], kiTs[p], load=False)
        S.finish()
    return nc


def build_attn0(nu=2, nblk=NBLK, lambda_init=0.2):
    nc = bass.Bass("TRN2", target_bir_lowering=False)
    tt = nblk * 128
    QT = nc.dram_tensor("QT", [nu, 128, tt], BF16, kind="ExternalInput").ap()
    KT = nc.dram_tensor("KT", [nu, 128, tt], BF16, kind="ExternalInput").ap()
    V = nc.dram_tensor("V", [nu, tt, 129], BF16, kind="ExternalInput").ap()
    lamv = nc.dram_tensor("lamv", [4, 64], F32, kind="ExternalInput").ap()
    subg = nc.dram_tensor("subg", [128], F32, kind="ExternalInput").ap()
    O = nc.dram_tensor("O", [nu, tt, 128], BF16, kind="ExternalOutput").ap()
    with ExitStack() as es:
        S = Sched(nc, es)
        trif = S.sbuf("trif", [128, 128], F32)
        tri = S.sbuf("tri", [128, 128], BF16)
        S.op("pool", lambda e: e.memset(trif[:], 1.0), writes=[trif])
        S.op("pool", lambda e: e.affine_select(out=trif[:], in_=trif[:], pattern=[[1, 128]], compare_op=ALU.is_ge,
                                               fill=0.0, base=0, channel_multiplier=-1), reads=[trif], writes=[trif])
        S.op("pool", lambda e: e.tensor_copy(out=tri[:], in_=trif[:]), reads=[trif], writes=[tri])
        lv = S.sbuf("lv", [128, 4, 64], F32)
        for i in range(4):
            S.dma("sp", lv[:, i, :], lamv[i, :].partition_broadcast(128), lv, load=True)
        gs = load_bcast(S, "gs", subg, 128)
        S.op("dve", lambda e: e.tensor_scalar(out=gs[:], in0=gs[:], scalar1=float(1.0 - lambda_init), scalar2=None, op0=ALU.mult),
             reads=[gs], writes=[gs])
        lt = S.sbuf("lt", [128, 2, 64], F32)
        ls = S.sbuf("ls", [128, 8], F32)
        S.op("dve", lambda e: e.tensor_tensor(out=lt[:, 0, :], in0=lv[:, 0, :], in1=lv[:, 1, :], op=ALU.mult), reads=[lv], writes=[lt])
        S.op("dve", lambda e: e.tensor_tensor(out=lt[:, 1, :], in0=lv[:, 2, :], in1=lv[:, 3, :], op=ALU.mult), reads=[lv, lt], writes=[lt])
        S.op("dve", lambda e: e.tensor_reduce(out=ls[:, 0:2], in_=lt[:], axis=AX.X, op=ALU.add), reads=[lt], writes=[ls])
        S.op("act", lambda e: e.activation(out=ls[:, 2:4], in_=ls[:, 0:2], func=AF.Exp), reads=[ls], writes=[ls])
        S.op("dve", lambda e: e.tensor_tensor(out=ls[:, 4:5], in0=ls[:, 2:3], in1=ls[:, 3:4], op=ALU.subtract), reads=[ls], writes=[ls])
        S.op("dve", lambda e: e.tensor_scalar(out=ls[:, 5:6], in0=ls[:, 4:5], scalar1=float(lambda_init), scalar2=-1.0,
                                              op0=ALU.add, op1=ALU.mult), reads=[ls], writes=[ls])
        qt_sb = S.sbuf("qt_sb", [128, tt], BF16)
        kt_sb = S.sbuf("kt_sb", [128, tt], BF16)
        v_sb = S.sbuf("v_sb", [128, nblk, 129], BF16)
        ps = [S.psum("ps%d" % i, [128, 512], F32) for i in range(4)]
        acc = [S.psum("acc%d" % i, [128, 512], F32) for i in range(3)]
        pt = [S.sbuf("pt%d" % i, [128, 512], BF16) for i in range(4)]
        fst = [S.sbuf("fst%d" % i, [128, 8], F32) for i in range(2)]
        o0 = [S.sbuf("o0_%d" % i, [128, 128], F32) for i in range(2)]
        o1 = [S.sbuf("o1_%d" % i, [128, 128], F32) for i in range(2)]
        fj = S.sbuf("fj", [128, 128], F32)
        ob = [S.sbuf("ob%d" % i, [128, 128], BF16) for i in range(2)]

        def accv(c, s):
            idx = c * 4 + s
            return acc[idx // 3], acc[idx // 3][:, (idx % 3) * 129:(idx % 3) * 129 + 129]

        fin_cnt = 0
        for u in range(nu):
            nchunk = 4
            csz = (tt + nchunk - 1) // nchunk
            for ci in range(nchunk):
                a, b_ = ci * csz, min(tt, (ci + 1) * csz)
                S.dma("sp", kt_sb[:, a:b_], KT[u, :, a:b_], kt_sb, load=True)
                S.dma("sp", qt_sb[:, a:b_], QT[u, :, a:b_], qt_sb, load=True)
            vsrc = V[u].rearrange("(j p) e -> p j e", p=128)
            for j0 in range(0, nblk, 16):
                j1 = min(nblk, j0 + 16)
                S.dma("sp", v_sb[:, j0:j1, :], vsrc[:, j0:j1, :], v_sb, load=True)
            ntile = (nblk + 3) // 4
            for qt in range(ntile):
                qb0 = qt * 4
                nsub = min(4, nblk - qb0)
                q0 = qb0 * 128
                steps = [(c, j) for j in range(qb0 + nsub) for c in range(2)]
                n = len(steps)
                bank_started = [False, False, False]

                def emitS(i):
                    c, j = steps[i]
                    s0 = max(0, j - qb0)
                    pb = ps[i % 4]
                    S.op("pe", lambda e: e.matmul(pb[:, s0 * 128:nsub * 128],
                                                  lhsT=kt_sb[c * 64:(c + 1) * 64, j * 128:(j + 1) * 128],
                                                  rhs=qt_sb[c * 64:(c + 1) * 64, q0 + s0 * 128:q0 + nsub * 128],
                                                  start=True, stop=True),
                         reads=[kt_sb, qt_sb], writes=[pb])

                def emitE(i):
                    c, j = steps[i]
                    s0 = max(0, j - qb0)
                    pb = ps[i % 4]
                    pp = pt[i % 4]
                    S.op("act", lambda e: e.activation(out=pp[:, s0 * 128:nsub * 128], in_=pb[:, s0 * 128:nsub * 128], func=AF.Exp),
                         reads=[pb], writes=[pp])
                    if j >= qb0:
                        S.op("dve", lambda e: e.tensor_tensor(out=pp[:, s0 * 128:(s0 + 1) * 128], in0=pp[:, s0 * 128:(s0 + 1) * 128],
                                                              in1=tri[:], op=ALU.mult), reads=[pp, tri], writes=[pp])

                def emitPV(i):
                    c, j = steps[i]
                    s0 = max(0, j - qb0)
                    pp = pt[i % 4]
                    fns = []
                    wr = []
                    for s in range(s0, nsub):
                        ab, av = accv(c, s)
                        bi = (c * 4 + s) // 3
                        st = not bank_started[bi]
                        bank_started[bi] = True
                        if ab not in wr:
                            wr.append(ab)
                        fns.append(lambda e, s=s, av=av, st=st: e.matmul(av, lhsT=pp[:, s * 128:(s + 1) * 128], rhs=v_sb[:, j, :],
                                                                         start=st, stop=(j == qb0 + s), skip_group_check=True))
                    S.multi("pe", fns, reads=[pp, v_sb], writes=wr)

                LA = 2
                for i in range(min(LA, n)):
                    emitS(i)
                for i in range(n):
                    if i + LA < n:
                        emitS(i + LA)
                    emitE(i)
                    emitPV(i)
                for s in range(nsub):
                    f = fin_cnt % 2
                    fin_cnt += 1
                    a0b, a0 = accv(0, s)
                    a1b, a1 = accv(1, s)
                    st_ = fst[f]
                    S.op("dve", lambda e: e.reciprocal(out=st_[:, 0:1], in_=a0[:, 128:129]), reads=[a0b], writes=[st_])
                    S.op("dve", lambda e: e.reciprocal(out=st_[:, 1:2], in_=a1[:, 128:129]), reads=[a1b, st_], writes=[st_])
                    S.op("dve", lambda e: e.tensor_tensor(out=st_[:, 2:3], in0=st_[:, 1:2], in1=ls[:, 5:6], op=ALU.mult),
                         reads=[st_, ls], writes=[st_])
                    S.op("dve", lambda e: e.tensor_scalar(out=o0[f][:], in0=a0[:, 0:128], scalar1=st_[:, 0:1], scalar2=None, op0=ALU.mult),
                         reads=[a0b, st_], writes=[o0[f]])
                    S.op("dve", lambda e: e.scalar_tensor_tensor(out=o1[f][:], in0=a1[:, 0:128], scalar=st_[:, 2:3], in1=o0[f][:],
                                                                 op0=ALU.mult, op1=ALU.add), reads=[a1b, st_, o0[f]], writes=[o1[f]])
                    S.op("act", lambda e: e.activation(out=fj[:], in_=o1[f][:], func=AF.Square, accum_out=st_[:, 3:4]),
                         reads=[o1[f], st_], writes=[fj, st_])
                    S.op("act", lambda e: e.activation(out=st_[:, 4:5], in_=st_[:, 3:4], func=AF.Sqrt, scale=1.0 / 128, bias=EPS),
                         reads=[st_], writes=[st_])
                    S.op("dve", lambda e: e.reciprocal(out=st_[:, 5:6], in_=st_[:, 4:5]), reads=[st_], writes=[st_])
                    S.op("dve", lambda e: e.scalar_tensor_tensor(out=ob[f][:], in0=o1[f][:], scalar=st_[:, 5:6], in1=gs[:],
                                                                 op0=ALU.mult, op1=ALU.mult), reads=[o1[f], st_, gs], writes=[ob[f]])
                    S.dma("sp", O[u, q0 + s * 128:q0 + (s + 1) * 128, :], ob[f][:], ob[f], load=False)
        S.finish()
    return nc


def build_ffnp(final, nblk=CB):
    nc = bass.Bass("TRN2", target_bir_lowering=False)
    ct = nblk * 128
    h_in = nc.dram_tensor("h_in", [ct, D], F32, kind="ExternalInput").ap()
    o_in = nc.dram_tensor("o_in", [ct, D], BF16, kind="ExternalInput").ap()
    wo_in = nc.dram_tensor("wo_in", [D, D], F32, kind="ExternalInput").ap()
    g_in = nc.dram_tensor("g_in", [D], F32, kind="ExternalInput").ap()
    wup_in = nc.dram_tensor("wup_in", [D, 2 * FF], F32, kind="ExternalInput").ap()
    cpar_in = nc.dram_tensor("cpar_in", [128, 2 * NFC, 4], F32, kind="ExternalInput").ap()
    wdn_in = nc.dram_tensor("wdn_in", [FF, D], F32, kind="ExternalInput").ap()
    if final:
        gf_in = nc.dram_tensor("gf_in", [D], F32, kind="ExternalInput").ap()
    h_out = nc.dram_tensor("h_out", [ct, D], F32, kind="ExternalOutput").ap()
    with ExitStack() as es:
        S = Sched(nc, es)
        ident = make_ident(S, nc)
        g_bc = load_bcast(S, "g_bc", g_in, D)
        if final:
            gf_bc = load_bcast(S, "gf_bc", gf_in, D)
        cpar = S.sbuf("cpar", [128, 2 * NFC, 4], F32)
        S.dma("sp", cpar[:], cpar_in, cpar, load=True)
        wo = load_weight_bf16(S, "wo", wo_in, D)
        hres = S.sbuf("hres", [128, 4, D], F32)
        hnT = S.sbuf("hnT", [128, 8, 512], BF16)
        aT = S.sbuf("aT", [128, NFC, 512], BF16)
        wdn = S.sbuf("wdn", [128, NFC, D], BF16)
        wu = [S.sbuf("wu%d" % i, [128, 8, 2, 128], BF16) for i in range(4)]
        ob = [S.sbuf("ob%d" % i, [128, D], BF16) for i in range(2)]
        oT = [S.sbuf("oT%d" % i, [128, 8, 128], BF16) for i in range(2)]
        hn = S.sbuf("hn", [128, D], BF16)
        junk = S.sbuf("junk", [128, D], BF16)
        stat = S.sbuf("stat", [128, 8], F32)
        ub = [S.sbuf("ub%d" % i, [128, 514], F32) for i in range(4)]
        cg = [S.sbuf("cg%d" % i, [128, 512], F32) for i in range(4)]
        sg = [S.sbuf("sg%d" % i, [128, 512], F32) for i in range(2)]
        carry = S.sbuf("carry", [128, 2 * NFC, 2], F32)
        if final:
            fo = [S.sbuf("fo%d" % i, [128, D], F32) for i in range(2)]
        ptr = S.psum("ptr", [128, 8, 128], BF16)
        pY = [S.psum("pY%d" % i, [128, 512], F32) for i in range(2)]
        pU = [S.psum("pU%d" % i, [128, 512], F32) for i in range(4)]
        S.op("dve", lambda e: e.memset(carry[:], 0.0), writes=[carry])
        wup_v = wup_in.rearrange("(k p) (t f) -> p k t f", p=128, t=2)
        wdn_v = wdn_in.rearrange("(c p) n -> p c n", p=128)
        wup_s = nc.dram_tensor("wup_s", [NFC, 128, 2048], BF16, kind="Internal").ap()
        wdn_s = nc.dram_tensor("wdn_s", [128, NFC, D], BF16, kind="Internal").ap()
        wup_sb = Buf(None, "wup_scr")
        wdn_sb = Buf(None, "wdn_scr")
        for c0 in range(0, NFC, 6):
            c1 = min(NFC, c0 + 6)
            S.dma("pool", wdn_s[:, c0:c1, :], wdn_v[:, c0:c1, :], wdn_sb, load=True)
        for c in range(NFC):
            dstv = wup_s[c].rearrange("p (k t f) -> p k t f", k=8, t=2)
            for t in range(2):
                S.dma("pool", dstv[:, :, t, :], wup_v[:, :, t, c * 128:(c + 1) * 128], wup_sb, load=True)
        tiles = [(b0, min(4, nblk - b0)) for b0 in range(0, nblk, 4)]
        ycnt = 0
        for (b0, nb) in tiles:
            ntok = nb * 128
            for c0 in range(0, NFC, 11):
                c1 = min(NFC, c0 + 11)
                S.dma("sp", wdn[:, c0:c1, :], wdn_s[:, c0:c1, :], wdn, load=True, extra_reads=[wdn_sb])
            for s in range(nb):
                blk = b0 + s
                tsl = slice(blk * 128, (blk + 1) * 128)
                p = s % 2
                S.dma("sp", hres[:, s, :], h_in[tsl, :], hres, load=True)
            S.dma("sp", ob[0][:], o_in[b0 * 128:(b0 + 1) * 128, :], ob[0], load=True)
            for s in range(nb):
                p = s % 2
                if s + 1 < nb:
                    S.dma("sp", ob[1 - p][:], o_in[(b0 + s + 1) * 128:(b0 + s + 2) * 128, :], ob[1 - p], load=True)
                S.multi("pe", [(lambda e, k=k: e.transpose(ptr[:, k, :], ob[p][:, k * 128:(k + 1) * 128], ident[:])) for k in range(8)],
                        reads=[ob[p], ident], writes=[ptr])
                S.op("act", lambda e: e.copy(out=oT[p][:], in_=ptr[:]), reads=[ptr], writes=[oT[p]])
                for hf in range(2):
                    py = pY[ycnt % 2]
                    ycnt += 1
                    S.multi("pe", [(lambda e, k=k: e.matmul(py[:], lhsT=oT[p][:, k, :], rhs=wo[:, k, hf * 512:(hf + 1) * 512],
                                                            start=(k == 0), stop=(k == 7))) for k in range(8)],
                            reads=[oT[p], wo], writes=[py])
                    S.op("dve", lambda e: e.tensor_tensor(out=hres[:, s, hf * 512:(hf + 1) * 512], in0=py[:],
                                                          in1=hres[:, s, hf * 512:(hf + 1) * 512], op=ALU.add),
                         reads=[py, hres], writes=[hres])
            for s in range(nb):
                emit_rmsnorm_T(S, hres, hres[:, s, :], g_bc, hn, stat, junk, ptr, ident, hnT, hnT[:, :, s * 128:(s + 1) * 128])
            for c in range(NFC):
                w = wu[c % 4]
                S.dma("sp", w[:].rearrange("p k t f -> p (k t f)"), wup_s[c], w, load=True, extra_reads=[wup_sb])
                pg = pU[(2 * c) % 4]
                pv = pU[(2 * c + 1) % 4]
                for (pp, t) in ((pg, 0), (pv, 1)):
                    S.multi("pe", [(lambda e, k=k: e.matmul(pp[:, 0:ntok], lhsT=w[:, k, t, :], rhs=hnT[:, k, 0:ntok],
                                                            start=(k == 0), stop=(k == 7))) for k in range(8)],
                            reads=[w, hnT], writes=[pp])
                cvals = []
                for (pp, t) in ((pg, 0), (pv, 1)):
                    cc = t * NFC + c
                    u_ = ub[(2 * c + t) % 4]
                    cgb = cg[(2 * c + t) % 4]
                    S.op("act", lambda e: e.copy(out=u_[:, 2:2 + ntok], in_=pp[:, 0:ntok]), reads=[pp], writes=[u_])
                    S.op("dve", lambda e: e.tensor_copy(out=u_[:, 0:2], in_=carry[:, cc, :]), reads=[carry, u_], writes=[u_])
                    S.op("dve", lambda e: e.tensor_copy(out=carry[:, cc, :], in_=u_[:, ntok:ntok + 2]), reads=[u_, carry], writes=[carry])
                    S.op("pool", lambda e: e.tensor_scalar(out=cgb[:, 0:ntok], in0=u_[:, 2:2 + ntok], scalar1=cpar[:, cc, 2:3],
                                                           scalar2=cpar[:, cc, 3:4], op0=ALU.mult, op1=ALU.add),
                         reads=[u_, cpar], writes=[cgb])
                    S.op("dve", lambda e: e.scalar_tensor_tensor(out=cgb[:, 0:ntok], in0=u_[:, 1:1 + ntok], scalar=cpar[:, cc, 1:2],
                                                                 in1=cgb[:, 0:ntok], op0=ALU.mult, op1=ALU.add),
                         reads=[u_, cpar, cgb], writes=[cgb])
                    S.op("dve", lambda e: e.scalar_tensor_tensor(out=cgb[:, 0:ntok], in0=u_[:, 0:ntok], scalar=cpar[:, cc, 0:1],
                                                                 in1=cgb[:, 0:ntok], op0=ALU.mult, op1=ALU.add),
                         reads=[u_, cpar, cgb], writes=[cgb])
                    cvals.append(cgb)
                sgb = sg[c % 2]
                S.op("act", lambda e: e.activation(out=sgb[:, 0:ntok], in_=cvals[0][:, 0:ntok], func=AF.Silu),
                     reads=[cvals[0]], writes=[sgb])
                S.op("pool", lambda e: e.tensor_tensor(out=aT[:, c, 0:ntok], in0=sgb[:, 0:ntok], in1=cvals[1][:, 0:ntok], op=ALU.mult),
                     reads=[sgb, cvals[1]], writes=[aT])
            for s in range(nb):
                for hf in range(2):
                    py = pY[ycnt % 2]
                    ycnt += 1
                    S.multi("pe", [(lambda e, c=c: e.matmul(py[:], lhsT=aT[:, c, s * 128:(s + 1) * 128],
                                                            rhs=wdn[:, c, hf * 512:(hf + 1) * 512],
                                                            start=(c == 0), stop=(c == NFC - 1))) for c in range(NFC)],
                            reads=[aT, wdn], writes=[py])
                    S.op("dve", lambda e: e.tensor_tensor(out=hres[:, s, hf * 512:(hf + 1) * 512], in0=py[:],
                                                          in1=hres[:, s, hf * 512:(hf + 1) * 512], op=ALU.add),
                         reads=[py, hres], writes=[hres])
            for s in range(nb):
                blk = b0 + s
                tsl = slice(blk * 128, (blk + 1) * 128)
                if final:
                    f = fo[s % 2]
                    S.op("act", lambda e: e.activation(out=junk[:], in_=hres[:, s, :], func=AF.Square, accum_out=stat[:, 0:1]),
                         reads=[hres], writes=[junk, stat])
                    S.op("act", lambda e: e.activation(out=stat[:, 1:2], in_=stat[:, 0:1], func=AF.Sqrt, scale=1.0 / D, bias=EPS),
                         reads=[stat], writes=[stat])
                    S.op("dve", lambda e: e.reciprocal(out=stat[:, 2:3], in_=stat[:, 1:2]), reads=[stat], writes=[stat])
                    S.op("dve", lambda e: e.scalar_tensor_tensor(out=f[:], in0=hres[:, s, :], scalar=stat[:, 2:3], in1=gf_bc[:],
                                                                 op0=ALU.mult, op1=ALU.mult), reads=[hres, stat, gf_bc], writes=[f])
                    S.dma("sp", h_out[tsl, :], f[:], f, load=False)
                else:
                    S.dma("sp", h_out[tsl, :], hres[:, s, :], hres, load=False)
        S.finish()
    return nc


NIT = 33
NKB = 132


def build_dsa_mask(nit=NIT):
    nc = bass.Bass("TRN2", target_bir_lowering=False)
    nkb = 4 * nit
    tot = 2 * nit * (nit + 1)
    kiT2 = nc.dram_tensor("kiT2", [128, nkb * 128], BF16, kind="ExternalInput").ap()
    qiT = nc.dram_tensor("qiT", [nit, 128, 4, 128], BF16, kind="ExternalInput").ap()
    wi = nc.dram_tensor("wi", [nit, 128, 8], F32, kind="ExternalInput").ap()
    cmask = nc.dram_tensor("cmask", [128, 512], F32, kind="ExternalInput").ap()
    maskT = nc.dram_tensor("maskT", [128, tot, 128], BF16, kind="ExternalOutput").ap()
    with ExitStack() as es:
        S = Sched(nc, es)
        identf = S.sbuf("identf", [128, 128], F32)
        ident = S.sbuf("ident", [128, 128], BF16)
        S.op("pool", lambda e: e.memset(identf[:], 0.0), writes=[identf])
        S.op("pool", lambda e: e.affine_select(out=identf[:], in_=identf[:], pattern=[[-1, 128]], compare_op=ALU.not_equal,
                                               fill=1.0, base=0, channel_multiplier=1), reads=[identf], writes=[identf])
        S.op("pool", lambda e: e.tensor_copy(out=ident[:], in_=identf[:]), reads=[identf], writes=[ident])
        ki = S.sbuf("ki", [128, nkb * 128], BF16)
        for a in range(0, nkb * 128, 4224):
            b_ = min(nkb * 128, a + 4224)
            S.dma("sp", ki[:, a:b_], kiT2[:, a:b_], ki, load=True)
        cm = S.sbuf("cm", [128, 512], F32)
        S.dma("sp", cm[:], cmask, cm, load=True)
        half = S.sbuf("half", [128, 1], F32)
        S.op("dve", lambda e: e.memset(half[:], 0.5), writes=[half])
        sc = S.sbuf("sc", [128, nkb * 128], F32)
        m01 = S.sbuf("m01", [128, nkb * 128], BF16)
        aj = S.sbuf("aj", [128, nkb * 64 + 128], BF16)
        qb = [S.sbuf("qb%d" % i, [128, 4, 128], BF16) for i in range(2)]
        wb = [S.sbuf("wb%d" % i, [128, 8], F32) for i in range(2)]
        dg = [S.sbuf("dg%d" % i, [128, 8, 128], BF16) for i in range(2)]
        rl = [S.sbuf("rl%d" % i, [128, 512], BF16) for i in range(4)]
        pI = [S.psum("pI%d" % i, [128, 512], F32) for i in range(4)]
        pA = [S.psum("pA%d" % i, [128, 512], F32) for i in range(2)]
        ptr = [S.psum("ptr%d" % i, [128, 8, 128], BF16) for i in range(2)]
        stg = [S.sbuf("stg%d" % i, [128, 8, 128], BF16) for i in range(2)]
        bs = S.sbuf("bs", [128, 16], F32)
        nm = S.sbuf("nm", [128, 1], F32)
        as_ = S.sbuf("as_", [128, 1], F32)
        step = 0
        tcnt = 0
        gcnt = 0
        for m in range(nit):
            p = m % 2
            nk = 4 * m + 4
            tc_ = nk * 128
            off = 2 * m * (m + 1)
            S.dma("sp", qb[p][:], qiT[m], qb[p], load=True)
            S.dma("sp", wb[p][:], wi[m], wb[p], load=True)
            S.op("dve", lambda e: e.tensor_tensor(out=dg[p][:], in0=identf[:].unsqueeze(1).broadcast_to([128, 8, 128]),
                                                  in1=wb[p][:].unsqueeze(2).broadcast_to([128, 8, 128]), op=ALU.mult),
                 reads=[identf, wb[p]], writes=[dg[p]])
            seq = [(g, h) for g in range(m + 1) for h in range(8)]
            n = len(seq)

            def emitR(i):
                g, h = seq[i]
                pi = pI[(step + i) % 4]
                pr = slice((h % 2) * 64, (h % 2) * 64 + 64)
                S.op("pe", lambda e: e.matmul(pi[:], lhsT=qb[p][pr, h // 2, :], rhs=ki[pr, g * 512:(g + 1) * 512], start=True, stop=True),
                     reads=[qb[p], ki], writes=[pi])

            def emitA(i):
                g, h = seq[i]
                pi = pI[(step + i) % 4]
                r_ = rl[(step + i) % 4]
                pa = pA[(gcnt + g) % 2]
                S.op("act", lambda e: e.activation(out=r_[:], in_=pi[:], func=AF.Relu), reads=[pi], writes=[r_])
                S.op("pe", lambda e: e.matmul(pa[:], lhsT=dg[p][:, h, :], rhs=r_[:], start=(h == 0), stop=(h == 7)),
                     reads=[dg[p], r_], writes=[pa])
                if h == 7:
                    S.op("act", lambda e: e.copy(out=sc[:, g * 512:(g + 1) * 512], in_=pa[:]), reads=[pa], writes=[sc])

            LA = 2
            for i in range(min(LA, n)):
                emitR(i)
            for i in range(n):
                if i + LA < n:
                    emitR(i + LA)
                emitA(i)
            step += n
            gcnt += m + 1
            S.op("dve", lambda e: e.tensor_reduce(out=bs[:, 0:1], in_=sc[:, 16:tc_], axis=AX.X, op=ALU.min), reads=[sc], writes=[bs])
            S.op("dve", lambda e: e.tensor_reduce(out=bs[:, 1:2], in_=sc[:, 16:tc_], axis=AX.X, op=ALU.max), reads=[sc, bs], writes=[bs])
            S.op("dve", lambda e: e.tensor_scalar(out=bs[:, 0:1], in0=bs[:, 0:1], scalar1=-1.0, scalar2=None, op0=ALU.add), reads=[bs], writes=[bs])
            S.op("dve", lambda e: e.tensor_scalar(out=bs[:, 1:2], in0=bs[:, 1:2], scalar1=1.0, scalar2=None, op0=ALU.add), reads=[bs], writes=[bs])
            S.op("dve", lambda e: e.memset(sc[:, 0:16], BIGM), reads=[sc], writes=[sc])
            S.op("dve", lambda e: e.tensor_tensor(out=sc[:, tc_ - 512:tc_], in0=sc[:, tc_ - 512:tc_], in1=cm[:], op=ALU.add),
                 reads=[sc, cm], writes=[sc])
            hf_ = (nk // 2) * 128
            nact = tc_ - hf_
            thr_c = float(TOPK) - 0.5 - 0.5 * nact
            for it in range(N_BISECT):
                S.op("dve", lambda e: e.scalar_tensor_tensor(out=bs[:, 2:3], in0=bs[:, 0:1], scalar=bs[:, 1:2], in1=half[:],
                                                             op0=ALU.add, op1=ALU.mult), reads=[bs, half], writes=[bs])
                S.op("dve", lambda e: e.tensor_scalar(out=nm[:], in0=bs[:, 2:3], scalar1=-1.0, scalar2=None, op0=ALU.mult),
                     reads=[bs], writes=[nm])
                S.op("act", lambda e: e.activation(out=aj[:, 0:nact], in_=sc[:, hf_:tc_], func=AF.Sign, bias=nm[:], scale=1.0,
                                                   accum_out=as_[:]), reads=[sc, nm], writes=[aj, as_])
                S.op("dve", lambda e: e.tensor_scalar(out=m01[:, 0:hf_], in0=sc[:, 0:hf_], scalar1=bs[:, 2:3], scalar2=None,
                                                      op0=ALU.is_ge, op1=ALU.add, accum_out=bs[:, 3:4]),
                     reads=[sc, bs], writes=[m01, bs])
                S.op("dve", lambda e: e.scalar_tensor_tensor(out=bs[:, 8:9], in0=as_[:], scalar=half[:], in1=bs[:, 3:4],
                                                             op0=ALU.mult, op1=ALU.add), reads=[as_, half, bs], writes=[bs])
                S.op("dve", lambda e: e.tensor_scalar(out=bs[:, 4:5], in0=bs[:, 8:9], scalar1=thr_c, scalar2=None, op0=ALU.is_ge),
                     reads=[bs], writes=[bs])
                S.op("dve", lambda e: e.tensor_tensor(out=bs[:, 5:6], in0=bs[:, 2:3], in1=bs[:, 0:1], op=ALU.subtract), reads=[bs], writes=[bs])
                S.op("dve", lambda e: e.tensor_tensor(out=bs[:, 6:7], in0=bs[:, 1:2], in1=bs[:, 2:3], op=ALU.subtract), reads=[bs], writes=[bs])
                S.op("dve", lambda e: e.scalar_tensor_tensor(out=bs[:, 0:1], in0=bs[:, 5:6], scalar=bs[:, 4:5], in1=bs[:, 0:1],
                                                             op0=ALU.mult, op1=ALU.add), reads=[bs], writes=[bs])
                S.op("dve", lambda e: e.scalar_tensor_tensor(out=bs[:, 1:2], in0=bs[:, 6:7], scalar=bs[:, 4:5], in1=bs[:, 2:3],
                                                             op0=ALU.mult, op1=ALU.add), reads=[bs], writes=[bs])
            S.op("dve", lambda e: e.tensor_scalar(out=m01[:, 0:tc_], in0=sc[:, 0:tc_], scalar1=bs[:, 0:1], scalar2=None, op0=ALU.is_ge),
                 reads=[sc, bs, m01], writes=[m01])
            for j0 in range(0, nk, 8):
                nb_ = min(8, nk - j0)
                pt_ = ptr[tcnt % 2]
                sg_ = stg[tcnt % 2]
                tcnt += 1
                S.multi("pe", [(lambda e, j=j: e.transpose(pt_[:, j, :], m01[:, (j0 + j) * 128:(j0 + j + 1) * 128], ident[:]))
                               for j in range(nb_)], reads=[m01, ident], writes=[pt_])
                S.op("act", lambda e: e.copy(out=sg_[:, 0:nb_, :], in_=pt_[:, 0:nb_, :]), reads=[pt_], writes=[sg_])
                S.dma("sp", maskT[:, off + j0:off + j0 + nb_, :], sg_[:, 0:nb_, :], sg_, load=False)
        S.finish()
    return nc


def build_dsa_attn(nblk=NBLK):
    nc = bass.Bass("TRN2", target_bir_lowering=False)
    tt = nblk * 128
    tot = nblk * (nblk + 1) // 2
    QT = nc.dram_tensor("QT", [128, 2, tt], BF16, kind="ExternalInput").ap()
    KT = nc.dram_tensor("KT", [128, 2, tt], BF16, kind="ExternalInput").ap()
    V = nc.dram_tensor("V", [tt, 4, 65], BF16, kind="ExternalInput").ap()
    MT = nc.dram_tensor("MT", [128, tot, 128], BF16, kind="ExternalInput").ap()
    O = nc.dram_tensor("O", [tt, 4, 64], BF16, kind="ExternalOutput").ap()
    with ExitStack() as es:
        S = Sched(nc, es)
        kt = S.sbuf("kt", [128, 2, tt], BF16)
        for c in range(2):
            for a in range(0, tt, 4224):
                b_ = min(tt, a + 4224)
                S.dma("sp", kt[:, c, a:b_], KT[:, c, a:b_], kt, load=True)
        v_sb = S.sbuf("v_sb", [128, nblk, 260], BF16)
        vsrc = V.rearrange("(j p) h e -> p j (h e)", p=128)
        for j0 in range(0, nblk, 16):
            j1 = min(nblk, j0 + 16)
            S.dma("sp", v_sb[:, j0:j1, :], vsrc[:, j0:j1, :], v_sb, load=True)
        qb = [S.sbuf("qb%d" % i, [128, 2, 128], BF16) for i in range(2)]
        mp = [S.sbuf("mp%d" % i, [128, 16, 128], BF16) for i in range(3)]
        pS = [S.psum("pS%d" % i, [128, 512], F32) for i in range(4)]
        acc = [S.psum("acc%d" % i, [128, 512], F32) for i in range(2)]
        pt = [S.sbuf("pt%d" % i, [128, 512], BF16) for i in range(4)]
        r4 = [S.sbuf("r4_%d" % i, [128, 4], F32) for i in range(2)]
        ob = [S.sbuf("ob%d" % i, [128, 4, 64], BF16) for i in range(2)]
        mcnt = 0
        gstep = 0
        for i in range(nblk):
            p = i % 2
            nk = i + 1
            off = i * (i + 1) // 2
            S.dma("sp", qb[p][:], QT[:, :, i * 128:(i + 1) * 128], qb[p], load=True)
            ab = acc[p]
            steps = []
            pieces = []
            for pc0 in range(0, nk, 16):
                pc1 = min(nk, pc0 + 16)
                pieces.append((pc0, pc1))
            started = [False]

            def load_piece(pi_):
                pc0, pc1 = pieces[pi_]
                mb = mp[(mcnt + pi_) % 3]
                S.dma("sp", mb[:, 0:pc1 - pc0, :], MT[:, off + pc0:off + pc1, :], mb, load=True)

            for pi_, (pc0, pc1) in enumerate(pieces):
                for hh in range(4):
                    for g0 in range(pc0, pc1, 4):
                        steps.append((pi_, hh, g0, min(pc1, g0 + 4)))
            n = len(steps)

            def emitS(si):
                pi_, hh, g0, g1 = steps[si]
                pb = pS[(gstep + si) % 4]
                pr = slice((hh % 2) * 64, (hh % 2) * 64 + 64)
                fns = []
                for jj, blk in enumerate(range(g0, g1)):
                    fns.append(lambda e, jj=jj, blk=blk: e.matmul(pb[:, jj * 128:(jj + 1) * 128],
                                                                  lhsT=kt[pr, hh // 2, blk * 128:(blk + 1) * 128],
                                                                  rhs=qb[p][pr, hh // 2, :], start=True, stop=True))
                S.multi("pe", fns, reads=[kt, qb[p]], writes=[pb])

            def emitE(si):
                pi_, hh, g0, g1 = steps[si]
                pc0, pc1 = pieces[pi_]
                w_ = (g1 - g0) * 128
                pb = pS[(gstep + si) % 4]
                pp = pt[(gstep + si) % 4]
                mb = mp[(mcnt + pi_) % 3]
                S.op("act", lambda e: e.activation(out=pp[:, 0:w_], in_=pb[:, 0:w_], func=AF.Exp), reads=[pb], writes=[pp])
                S.op("dve", lambda e: e.tensor_tensor(out=pp[:, 0:w_], in0=pp[:, 0:w_],
                                                      in1=mb[:, g0 - pc0:g1 - pc0, :].rearrange("p j q -> p (j q)"), op=ALU.mult),
                     reads=[pp, mb], writes=[pp])

            def emitPV(si):
                pi_, hh, g0, g1 = steps[si]
                pp = pt[(gstep + si) % 4]
                fns = []
                for jj, blk in enumerate(range(g0, g1)):
                    st = not started[0]
                    started[0] = True
                    fns.append(lambda e, jj=jj, blk=blk, st=st: e.matmul(ab[:, hh * 65:(hh + 1) * 65], lhsT=pp[:, jj * 128:(jj + 1) * 128],
                                                                         rhs=v_sb[:, blk, hh * 65:(hh + 1) * 65],
                                                                         start=st, stop=(blk == nk - 1), skip_group_check=True))
                S.multi("pe", fns, reads=[pp, v_sb], writes=[ab])

            LA = 2
            loaded = set()

            def need_piece(si):
                pi_ = steps[si][0]
                if pi_ not in loaded:
                    loaded.add(pi_)
                    load_piece(pi_)

            for si in range(min(LA, n)):
                need_piece(si)
                emitS(si)
            for si in range(n):
                if si + LA < n:
                    need_piece(si + LA)
                    emitS(si + LA)
                nxt = steps[si][0] + 1
                if nxt < len(pieces) and nxt not in loaded:
                    loaded.add(nxt)
                    load_piece(nxt)
                emitE(si)
                emitPV(si)
            gstep += n
            mcnt += len(pieces)
            S.op("dve", lambda e: e.reciprocal(out=r4[p][:], in_=ab[:, 0:260].rearrange("p (h e) -> p h e", e=65)[:, :, 64]),
                 reads=[ab], writes=[r4[p]])
            S.op("dve", lambda e: e.tensor_tensor(out=ob[p][:], in0=ab[:, 0:260].rearrange("p (h e) -> p h e", e=65)[:, :, 0:64],
                                                  in1=r4[p][:].unsqueeze(2).broadcast_to([128, 4, 64]), op=ALU.mult),
                 reads=[ab, r4[p]], writes=[ob[p]])
            S.dma("sp", O[i * 128:(i + 1) * 128, :, :], ob[p][:], ob[p], load=False)
        S.finish()
    return nc


_PROG = {}


def _prog(name, fn):
    if name not in _PROG:
        _PROG[name] = fn()
    return _PROG[name]


def _run(nc, in_maps):
    res = run_bass_kernel_spmd(nc, in_maps, core_ids=list(range(NCORE)))
    return res.results


def _rope_tables():
    inv = (np.float32(10000.0) ** (-(np.arange(0, 64, 2, dtype=np.float32)) / np.float32(64))).astype(np.float32)
    ang = (np.arange(T, dtype=np.float32)[:, None] * inv[None, :]).astype(np.float32)
    cos = np.cos(ang).astype(np.float32)
    sin = np.sin(ang).astype(np.float32)
    return np.stack([cos * np.float32(0.125), sin * np.float32(0.125), cos, sin], axis=1)


def _shard_rows(full, b, c):
    return np.ascontiguousarray(full[b, 32 * c * 128:(32 * c + CB) * 128])


def _cs_shard(cs_full, c):
    s = cs_full[32 * c * 128:(32 * c + CB) * 128]
    return np.ascontiguousarray(s.reshape(CB, 128, 4, 32).transpose(1, 0, 2, 3))


def _assemble_rows(shards, width_shape, dtype):
    out = np.zeros((B, T) + tuple(width_shape), dtype=dtype)
    for r in range(NCORE):
        b, c = divmod(r, 4)
        if c == 0:
            out[b, 0:CT] = shards[r]
        else:
            out[b, (32 * c + 1) * 128:(32 * c + CB) * 128] = shards[r][128:]
    return out


def _assemble_featmajor(shards, nch, prows=128):
    out = np.zeros((B, nch, prows, T), dtype=shards[0].dtype)
    for r in range(NCORE):
        b, c = divmod(r, 4)
        if c == 0:
            out[b, :, :, 0:CT] = shards[r]
        else:
            out[b, :, :, (32 * c + 1) * 128:(32 * c + CB) * 128] = shards[r][:, :, 128:]
    return out


def _cpar(conv_w, conv_b):
    c = np.concatenate([conv_w, conv_b[None, :]], axis=0)
    return np.ascontiguousarray(c.reshape(4, 2 * NFC, 128).transpose(2, 1, 0))


def kernel(x, meta_tokens, da_norm, da_w_qkv, da_lambda_q1, da_lambda_k1, da_lambda_q2, da_lambda_k2,
           da_subln, da_w_o, dsa_norm, dsa_w_in, dsa_idx_k_norm, dsa_w_o, ffn_norm, ffn_w_up,
           ffn_conv_w, ffn_conv_b, ffn_w_down, final_norm):
    f32 = np.float32
    x = np.asarray(x, f32)
    h0 = np.zeros((B, T, D), f32)
    h0[:, :NMETA] = np.asarray(meta_tokens, f32)[None]
    h0[:, NMETA:NMETA + L] = x
    cs_full = _rope_tables()
    A = lambda a: np.ascontiguousarray(np.asarray(a, f32))

    nc = _prog("projA", lambda: build_proj("A"))
    maps = []
    for r in range(NCORE):
        b, c = divmod(r, 4)
        maps.append({"h_in": _shard_rows(h0, b, c), "g_in": A(da_norm[0]), "w_in": A(da_w_qkv[0]), "cs_in": _cs_shard(cs_full, c)})
    res = _run(nc, maps)
    QT = _assemble_featmajor([r_["qT_o"] for r_ in res], 8)
    KT = _assemble_featmajor([r_["kT_o"] for r_ in res], 8)
    Vf = _assemble_rows([r_["v_o"] for r_ in res], (8, 129), NPBF)
    del res

    nc = _prog("attn0", lambda: build_attn0(2, NBLK, 0.2))
    lamv = np.stack([A(da_lambda_q1[0]), A(da_lambda_k1[0]), A(da_lambda_q2[0]), A(da_lambda_k2[0])])
    maps = []
    for r in range(NCORE):
        us = [divmod(2 * r + k, 8) for k in range(2)]
        maps.append({"QT": np.ascontiguousarray(np.stack([QT[b, h] for b, h in us])),
                     "KT": np.ascontiguousarray(np.stack([KT[b, h] for b, h in us])),
                     "V": np.ascontiguousarray(np.stack([Vf[b, :, h, :] for b, h in us])),
                     "lamv": lamv, "subg": A(da_subln[0])})
    res = _run(nc, maps)
    o1 = np.zeros((B, T, D), NPBF)
    for r in range(NCORE):
        for k in range(2):
            b, h = divmod(2 * r + k, 8)
            o1[b, :, h * 128:(h + 1) * 128] = res[r]["O"][k]
    del res, QT, KT, Vf

    nc = _prog("ffn0", lambda: build_ffnp(False))
    maps = []
    for r in range(NCORE):
        b, c = divmod(r, 4)
        maps.append({"h_in": _shard_rows(h0, b, c), "o_in": _shard_rows(o1, b, c), "wo_in": A(da_w_o[0]), "g_in": A(ffn_norm[0]),
                     "wup_in": A(ffn_w_up[0]), "cpar_in": _cpar(A(ffn_conv_w[0]), A(ffn_conv_b[0])), "wdn_in": A(ffn_w_down[0])})
    res = _run(nc, maps)
    h2 = _assemble_rows([r_["h_out"] for r_ in res], (D,), f32)
    del res, o1, h0

    nc = _prog("projB", lambda: build_proj("B"))
    maps = []
    for r in range(NCORE):
        b, c = divmod(r, 4)
        maps.append({"h_in": _shard_rows(h2, b, c), "g_in": A(dsa_norm[0]), "w_in": A(dsa_w_in[0]), "cs_in": _cs_shard(cs_full, c),
                     "idxg_in": A(dsa_idx_k_norm[0])})
    res = _run(nc, maps)
    QT = _assemble_featmajor([r_["qT_o"] for r_ in res], 8)
    KT = _assemble_featmajor([r_["kT_o"] for r_ in res], 8)
    Vf = _assemble_rows([r_["v_o"] for r_ in res], (16, 65), NPBF)
    QIT = _assemble_featmajor([r_["qiT_o"] for r_ in res], 4)
    KIT = _assemble_featmajor([r_["kiT_o"][None] for r_ in res], 1, 64)[:, 0]
    WI = _assemble_rows([r_["wi_o"] for r_ in res], (8,), f32)
    del res

    nc = _prog("dsamask", lambda: build_dsa_mask(NIT))
    maps = []
    kk = np.arange(512)[None, :]
    for r in range(NCORE):
        b, cp = divmod(r, 4)
        ki2 = np.zeros((128, NKB * 128), NPBF)
        ki2[0:64, :T] = KIT[b]
        ki2[64:128, :T] = KIT[b]
        qic = np.zeros((NIT, 128, 4, 128), NPBF)
        wic = np.zeros((NIT, 128, 8), f32)
        for m in range(NIT):
            i = 4 * m + cp
            if i < NBLK:
                qic[m] = QIT[b][:, :, i * 128:(i + 1) * 128].transpose(1, 0, 2)
                wic[m] = WI[b, i * 128:(i + 1) * 128]
        qq = np.arange(128)[:, None] + cp * 128
        cmask = np.where(kk <= qq, 0.0, -BIGC).astype(f32)
        maps.append({"kiT2": ki2, "qiT": qic, "wi": wic, "cmask": cmask})
    res = _run(nc, maps)
    tot = NBLK * (NBLK + 1) // 2
    MT = [np.zeros((128, tot, 128), NPBF) for _ in range(B)]
    for r in range(NCORE):
        b, cp = divmod(r, 4)
        mt = res[r]["maskT"]
        for m in range(NIT):
            i = 4 * m + cp
            if i < NBLK:
                offm = 2 * m * (m + 1)
                offi = i * (i + 1) // 2
                MT[b][:, offi:offi + i + 1, :] = mt[:, offm:offm + i + 1, :]
    del res, QIT, KIT, WI

    nc = _prog("dsaattn", lambda: build_dsa_attn(NBLK))
    maps = []
    for r in range(NCORE):
        b, hg = divmod(r, 4)
        maps.append({"QT": np.ascontiguousarray(QT[b, 2 * hg:2 * hg + 2].transpose(1, 0, 2)),
                     "KT": np.ascontiguousarray(KT[b, 2 * hg:2 * hg + 2].transpose(1, 0, 2)),
                     "V": np.ascontiguousarray(Vf[b, :, 4 * hg:4 * hg + 4, :]),
                     "MT": MT[b]})
    res = _run(nc, maps)
    o2 = np.zeros((B, T, D), NPBF)
    for r in range(NCORE):
        b, hg = divmod(r, 4)
        o2[b, :, hg * 256:(hg + 1) * 256] = res[r]["O"].reshape(T, 256)
    del res, QT, KT, Vf, MT

    nc = _prog("ffn1", lambda: build_ffnp(True))
    maps = []
    for r in range(NCORE):
        b, c = divmod(r, 4)
        maps.append({"h_in": _shard_rows(h2, b, c), "o_in": _shard_rows(o2, b, c), "wo_in": A(dsa_w_o[0]), "g_in": A(ffn_norm[1]),
                     "wup_in": A(ffn_w_up[1]), "cpar_in": _cpar(A(ffn_conv_w[1]), A(ffn_conv_b[1])), "wdn_in": A(ffn_w_down[1]),
                     "gf_in": A(final_norm)})
    res = _run(nc, maps)
    out = _assemble_rows([r_["h_out"] for r_ in res], (D,), f32)
    return np.ascontiguousarray(out[:, NMETA:NMETA + L])
```

```python
import math
from contextlib import ExitStack

import numpy as np
import ml_dtypes

import concourse.bass as bass
import concourse.mybir as mybir
from concourse.bass_utils import run_bass_kernel_spmd

F32 = mybir.dt.float32
BF16 = mybir.dt.bfloat16
AF = mybir.ActivationFunctionType
ALU = mybir.AluOpType
AX = mybir.AxisListType
NPBF = ml_dtypes.bfloat16

D = 1024
B = 2
L = 16384
NMETA = 16
T = 16512
NBLK = 129
NCORE = 8
CB = 33
CT = CB * 128
FF = 2816
NFC = 22
EPS = 1e-6
BIGM = 1.0e30
BIGC = 3.0e38
TOPK = 256
N_BISECT = 18


class Buf:
    def __init__(self, t, name=""):
        self.t = t
        self.name = name
        self.w = None
        self.r = {}
        self.dsem = None
        self.dcnt = 0

    def __getitem__(self, idx):
        return self.t[idx]


class Glob:
    def __init__(self, nc, es):
        self.nc = nc
        self.sem = {k: es.enter_context(nc.semaphore("sem_" + k)) for k in ("pe", "dve", "act", "pool", "sp")}
        self.cnt = {k: 0 for k in self.sem}
        self.seen = {k: {} for k in self.sem}
        self.pool = []
        self.es = es
        self.nalloc = 0
        self.io = {}

    def get_dsem(self):
        if self.pool:
            return self.pool.pop()
        self.nalloc += 1
        return (self.es.enter_context(self.nc.semaphore("dg%d" % self.nalloc)), 0)


class Sched:
    def __init__(self, nc, es, glob=None):
        self.nc = nc
        self.es = es
        self.glob = glob
        self.eng = {"pe": nc.tensor, "dve": nc.vector, "act": nc.scalar, "pool": nc.gpsimd, "sp": nc.sync}
        if glob is None:
            self.sem = {k: es.enter_context(nc.semaphore("sem_" + k)) for k in self.eng}
            self.cnt = {k: 0 for k in self.eng}
            self.seen = {k: {} for k in self.eng}
        else:
            self.sem, self.cnt, self.seen = glob.sem, glob.cnt, glob.seen
        self.nbuf = 0
        self.out_tokens = []
        self.dbufs = []
        if glob is not None:
            glob.phase = getattr(glob, "phase", 0) + 1
            self.pfx = "p%d_" % glob.phase
        else:
            self.pfx = ""

    def sbuf(self, name, shape, dt):
        t = self.es.enter_context(self.nc.sbuf_tensor(self.pfx + name, list(shape), dt))
        return Buf(t, name)

    def psum(self, name, shape, dt):
        t = self.es.enter_context(self.nc.psum_tensor(self.pfx + name, list(shape), dt))
        return Buf(t, name)

    def _wait(self, e, deps):
        eng = self.eng[e]
        for d in deps:
            if d is None:
                continue
            sem, val, owner = d
            if owner == e and e == "pe":
                continue
            key = id(sem)
            if self.seen[e].get(key, 0) >= val:
                continue
            eng.wait_ge(sem, val)
            self.seen[e][key] = val

    def _deps(self, reads, writes):
        deps = []
        for b in reads:
            deps.append(b.w)
        for b in writes:
            deps.append(b.w)
            deps.extend(b.r.values())
        return deps

    def op(self, e, fn, reads=(), writes=()):
        self._wait(e, self._deps(reads, writes))
        ins = fn(self.eng[e])
        self.cnt[e] += 1
        ins.then_inc(self.sem[e], 1)
        tok = (self.sem[e], self.cnt[e], e)
        for b in reads:
            b.r[e] = tok
        for b in writes:
            b.w = tok
            b.r = {}
        return ins

    def multi(self, e, fns, reads=(), writes=()):
        self._wait(e, self._deps(reads, writes))
        ins = None
        for fn in fns:
            ins = fn(self.eng[e])
        self.cnt[e] += 1
        ins.then_inc(self.sem[e], 1)
        tok = (self.sem[e], self.cnt[e], e)
        for b in reads:
            b.r[e] = tok
        for b in writes:
            b.w = tok
            b.r = {}
        return ins

    def dma(self, q, out, in_, sb, load=True, extra_reads=(), **kw):
        if load:
            deps = self._deps(extra_reads, [sb])
        else:
            deps = self._deps([sb] + list(extra_reads), [])
        self._wait(q, deps)
        if sb.dsem is None:
            if self.glob is None:
                sb.dsem = self.es.enter_context(self.nc.semaphore("d_" + sb.name))
            else:
                sb.dsem, sb.dcnt = self.glob.get_dsem()
            self.dbufs.append(sb)
        ins = self.eng[q].dma_start(out=out, in_=in_, **kw)
        sb.dcnt += 16
        ins.then_inc(sb.dsem, 16)
        tok = (sb.dsem, sb.dcnt, "dma")
        if load:
            sb.w = tok
            sb.r = {}
        else:
            sb.r["dma"] = tok
            self.out_tokens.append(tok)
        return ins

    def finish(self):
        last = {}
        for sem, val, owner in self.out_tokens:
            k = id(sem)
            if k not in last or last[k][1] < val:
                last[k] = (sem, val, owner)
        self._wait("sp", list(last.values()))
        for e in ("pe", "dve", "act", "pool"):
            if self.cnt[e] > 0:
                self._wait("sp", [(self.sem[e], self.cnt[e], e)])
        if self.glob is not None:
            allt = [(b.dsem, b.dcnt, "dma") for b in self.dbufs]
            for e in ("pe", "dve", "act", "pool"):
                self._wait(e, allt)
                for e2 in ("pe", "dve", "act", "pool"):
                    if e2 != e and self.cnt[e2] > 0:
                        self._wait(e, [(self.sem[e2], self.cnt[e2], e2)])
            self._wait("sp", allt)
            for b in self.dbufs:
                self.glob.pool.append((b.dsem, b.dcnt))
            self.dbufs = []


def _mknc(glob):
    return glob.nc if glob is not None else bass.Bass("TRN2", target_bir_lowering=False)


class _IO:
    def __init__(self, nc, glob):
        self.nc, self.glob = nc, glob

    def __call__(self, name, shape, dt, kind):
        if self.glob is not None:
            return self.glob.io[name]
        return self.nc.dram_tensor(name, list(shape), dt, kind=kind).ap()


def make_ident(S, nc):
    identf = S.sbuf("identf", [128, 128], F32)
    ident = S.sbuf("ident", [128, 128], BF16)
    S.op("pool", lambda e: e.memset(identf[:], 0.0), writes=[identf])
    S.op("pool", lambda e: e.affine_select(out=identf[:], in_=identf[:], pattern=[[-1, 128]],
                                           compare_op=ALU.not_equal, fill=1.0, base=0,
                                           channel_multiplier=1), reads=[identf], writes=[identf])
    S.op("pool", lambda e: e.tensor_copy(out=ident[:], in_=identf[:]), reads=[identf], writes=[ident])
    return ident


def load_weight_bf16(S, name, w_ap, ncols):
    wb = S.sbuf(name, [128, 8, ncols], BF16)
    src = w_ap.rearrange("(k p) n -> p k n", p=128)
    for k in range(8):
        S.dma("pool", wb[:, k, :], src[:, k, :], wb, load=True)
    return wb


def load_bcast(S, name, vec_ap, n, q="sp"):
    t = S.sbuf(name, [128, n], F32)
    S.dma(q, t[:], vec_ap.partition_broadcast(128), t, load=True)
    return t


def emit_rmsnorm_T(S, x_buf, x_ap, g_bc, hn, stat, junk, ptr, ident, dst_buf, dst_ap, ncol=1024):
    nch = ncol // 128
    S.op("act", lambda e: e.activation(out=junk[:, 0:ncol], in_=x_ap, func=AF.Square, accum_out=stat[:, 0:1]),
         reads=[x_buf], writes=[junk, stat])
    S.op("act", lambda e: e.activation(out=stat[:, 1:2], in_=stat[:, 0:1], func=AF.Sqrt, scale=1.0 / ncol, bias=EPS),
         reads=[stat], writes=[stat])
    S.op("dve", lambda e: e.reciprocal(out=stat[:, 2:3], in_=stat[:, 1:2]), reads=[stat], writes=[stat])
    S.op("dve", lambda e: e.scalar_tensor_tensor(out=hn[:, 0:ncol], in0=x_ap, scalar=stat[:, 2:3], in1=g_bc[:, 0:ncol],
                                                 op0=ALU.mult, op1=ALU.mult),
         reads=[x_buf, stat, g_bc], writes=[hn])
    S.multi("pe", [(lambda e, k=k: e.transpose(ptr[:, k, :], hn[:, k * 128:(k + 1) * 128], ident[:])) for k in range(nch)],
            reads=[hn, ident], writes=[ptr])
    S.op("act", lambda e: e.copy(out=dst_ap, in_=ptr[:, 0:nch, :]), reads=[ptr], writes=[dst_buf])


def emit_rope(S, src_buf, src_ap3, cos_ap, sin_ap, tmps, out_buf, out_ap3, nh):
    t1, t2, t3, t4 = tmps
    if nh > 1:
        cb = cos_ap.unsqueeze(1).broadcast_to([128, nh, 32])
        sb = sin_ap.unsqueeze(1).broadcast_to([128, nh, 32])
        v = lambda t: t[:, 0:nh * 32].rearrange("p (h d) -> p h d", d=32)
    else:
        cb, sb = cos_ap, sin_ap
        v = lambda t: t[:, 0:32]
        src_ap3 = src_ap3
    x1 = src_ap3[:, :, 0:32] if nh > 1 else src_ap3[:, 0:32]
    x2 = src_ap3[:, :, 32:64] if nh > 1 else src_ap3[:, 32:64]
    o1 = out_ap3[:, :, 0:32] if nh > 1 else out_ap3[:, 0:32]
    o2 = out_ap3[:, :, 32:64] if nh > 1 else out_ap3[:, 32:64]
    S.op("dve", lambda e: e.tensor_tensor(out=v(t1), in0=x1, in1=cb, op=ALU.mult), reads=[src_buf], writes=[t1])
    S.op("dve", lambda e: e.tensor_tensor(out=v(t2), in0=x2, in1=sb, op=ALU.mult), reads=[src_buf], writes=[t2])
    S.op("dve", lambda e: e.tensor_tensor(out=v(t3), in0=x2, in1=cb, op=ALU.mult), reads=[src_buf], writes=[t3])
    S.op("dve", lambda e: e.tensor_tensor(out=v(t4), in0=x1, in1=sb, op=ALU.mult), reads=[src_buf], writes=[t4])
    S.op("pool", lambda e: e.tensor_tensor(out=o1, in0=v(t1), in1=v(t2), op=ALU.subtract), reads=[t1, t2], writes=[out_buf])
    S.op("pool", lambda e: e.tensor_tensor(out=o2, in0=v(t3), in1=v(t4), op=ALU.add), reads=[t3, t4, out_buf], writes=[out_buf])


def build_proj(kind, nblk=CB, glob=None):
    nc = _mknc(glob)
    IO = _IO(nc, glob)
    ncols = 3072 if kind == "A" else 3656
    ct = nblk * 128
    h_in = IO("h_in", [ct, D], F32, "ExternalInput")
    g_in = IO("g_in", [D], F32, "ExternalInput")
    w_in = IO("w_in", [D, ncols], F32, "ExternalInput")
    cs_in = IO("cs_in", [128, nblk, 4, 32], F32, "ExternalInput")
    qT_o = IO("qT_o", [8, 128, ct], BF16, "ExternalOutput")
    kT_o = IO("kT_o", [8, 128, ct], BF16, "ExternalOutput")
    if kind == "A":
        v_o = IO("v_o", [ct, 8, 129], BF16, "ExternalOutput")
    else:
        v_o = IO("v_o", [ct, 16, 65], BF16, "ExternalOutput")
        idxg_in = IO("idxg_in", [64], F32, "ExternalInput")
        qiT_o = IO("qiT_o", [4, 128, ct], BF16, "ExternalOutput")
        kiT_o = IO("kiT_o", [64, ct], BF16, "ExternalOutput")
        wi_o = IO("wi_o", [ct, 8], F32, "ExternalOutput")
    with ExitStack() as es:
        S = Sched(nc, es, glob)
        ident = make_ident(S, nc)
        g_bc = load_bcast(S, "g_bc", g_in, D)
        csb = [S.sbuf("csb%d" % i, [128, 4, 32], F32) for i in range(2)]
        wb = load_weight_bf16(S, "wb", w_in, ncols)
        if kind == "B":
            idxg = load_bcast(S, "idxg", idxg_in, 64)
        hb = [S.sbuf("hb%d" % i, [128, D], F32) for i in range(2)]
        hn = [S.sbuf("hn%d" % i, [128, D], BF16) for i in range(2)]
        stat = [S.sbuf("stat%d" % i, [128, 8], F32) for i in range(2)]
        junk = S.sbuf("junk", [128, D], BF16)
        hnT = [S.sbuf("hnT%d" % i, [128, 8, 128], BF16) for i in range(2)]
        ptr = [S.psum("ptr%d" % i, [128, 8, 128], BF16) for i in range(1)]
        pA = S.psum("pA", [128, 1024], F32)
        pB = S.psum("pB", [128, 1024], F32)
        pC = S.psum("pC", [128, 1024], F32)
        if kind == "B":
            pD = S.psum("pD", [128, 512], F32)
        tq = [S.sbuf("tq%d" % i, [128, 512], F32) for i in range(4)]
        tk = [S.sbuf("tk%d" % i, [128, 512], F32) for i in range(4)]
        qr = [S.sbuf("qr%d" % i, [128, 1024], BF16) for i in range(2)]
        kr = [S.sbuf("kr%d" % i, [128, 1024], BF16) for i in range(2)]
        qTs = [S.sbuf("qTs%d" % i, [128, 8, 128], BF16) for i in range(2)]
        kTs = [S.sbuf("kTs%d" % i, [128, 8, 128], BF16) for i in range(2)]
        if kind == "A":
            vs = [S.sbuf("vs%d" % i, [128, 8, 129], BF16) for i in range(2)]
        else:
            vs = [S.sbuf("vs%d" % i, [128, 16, 65], BF16) for i in range(2)]
            tqi = [S.sbuf("tqi%d" % i, [128, 256], F32) for i in range(4)]
            qir = [S.sbuf("qir%d" % i, [128, 512], BF16) for i in range(2)]
            qiTs = [S.sbuf("qiTs%d" % i, [128, 4, 128], BF16) for i in range(2)]
            kin = S.sbuf("kin", [128, 64], F32)
            tki = [S.sbuf("tki%d" % i, [128, 32], F32) for i in range(4)]
            kir = [S.sbuf("kir%d" % i, [128, 64], BF16) for i in range(2)]
            kiTs = [S.sbuf("kiTs%d" % i, [64, 128], BF16) for i in range(2)]
            wis = [S.sbuf("wis%d" % i, [128, 8], F32) for i in range(2)]
            kstat = S.sbuf("kstat", [128, 8], F32)
            kjunk = S.sbuf("kjunk", [128, 64], F32)
        for v in vs:
            S.op("pool", lambda e, v=v: e.memset(v[:], 1.0), writes=[v])

        def proj(pbuf, col0, n, hT):
            fns = []
            for c0 in range(0, n, 512):
                cw = min(512, n - c0)
                for k in range(8):
                    fns.append(lambda e, c0=c0, cw=cw, k=k: e.matmul(
                        pbuf[:, c0:c0 + cw], lhsT=hT[:, k, :], rhs=wb[:, k, col0 + c0:col0 + c0 + cw],
                        start=(k == 0), stop=(k == 7)))
            S.multi("pe", fns, reads=[hT, wb], writes=[pbuf])

        for i in range(nblk):
            p = i % 2
            tsl = slice(i * 128, (i + 1) * 128)
            S.dma("sp", hb[p][:], h_in[tsl, :], hb[p], load=True)
            cs = csb[p]
            S.dma("sp", cs[:], cs_in[:, i, :, :], cs, load=True)
            emit_rmsnorm_T(S, hb[p], hb[p][:], g_bc, hn[p], stat[p], junk, ptr[0], ident, hnT[p], hnT[p][:])
            proj(pA, 0, 1024, hnT[p])
            proj(pB, 1024, 1024, hnT[p])
            proj(pC, 2048, 1024, hnT[p])
            emit_rope(S, pA, pA[:].rearrange("p (h d) -> p h d", d=64), cs[:, 0, :], cs[:, 1, :], tq,
                      qr[p], qr[p][:].rearrange("p (h d) -> p h d", d=64), 16)
            emit_rope(S, pB, pB[:].rearrange("p (h d) -> p h d", d=64), cs[:, 2, :], cs[:, 3, :], tk,
                      kr[p], kr[p][:].rearrange("p (h d) -> p h d", d=64), 16)
            if kind == "A":
                S.op("act", lambda e: e.copy(out=vs[p][:, :, 0:128], in_=pC[:].rearrange("p (h d) -> p h d", d=128)),
                     reads=[pC], writes=[vs[p]])
            else:
                S.op("act", lambda e: e.copy(out=vs[p][:, :, 0:64], in_=pC[:].rearrange("p (h d) -> p h d", d=64)),
                     reads=[pC], writes=[vs[p]])
            S.dma("sp", v_o[tsl, :, :], vs[p][:], vs[p], load=False)
            for (src, stg, dst) in ((qr[p], qTs[p], qT_o), (kr[p], kTs[p], kT_o)):
                S.multi("pe", [(lambda e, k=k, src=src: e.transpose(ptr[0][:, k, :], src[:, k * 128:(k + 1) * 128], ident[:]))
                               for k in range(8)], reads=[src, ident], writes=[ptr[0]])
                S.op("act", lambda e, stg=stg: e.copy(out=stg[:], in_=ptr[0][:]), reads=[ptr[0]], writes=[stg])
                S.dma("sp", dst[:, :, tsl].rearrange("c p t -> p c t"), stg[:], stg, load=False)
            if kind == "B":
                proj(pD, 3072, 512, hnT[p])
                emit_rope(S, pD, pD[:].rearrange("p (h d) -> p h d", d=64), cs[:, 0, :], cs[:, 1, :], tqi,
                          qir[p], qir[p][:].rearrange("p (h d) -> p h d", d=64), 8)
                S.multi("pe", [(lambda e, k=k: e.transpose(ptr[0][:, k, :], qir[p][:, k * 128:(k + 1) * 128], ident[:]))
                               for k in range(4)], reads=[qir[p], ident], writes=[ptr[0]])
                S.op("act", lambda e: e.copy(out=qiTs[p][:], in_=ptr[0][:, 0:4, :]), reads=[ptr[0]], writes=[qiTs[p]])
                S.dma("sp", qiT_o[:, :, tsl].rearrange("c p t -> p c t"), qiTs[p][:], qiTs[p], load=False)
                proj(pD, 3584, 72, hnT[p])
                S.op("act", lambda e: e.activation(out=kjunk[:], in_=pD[:, 0:64], func=AF.Square, accum_out=kstat[:, 0:1]),
                     reads=[pD], writes=[kjunk, kstat])
                S.op("act", lambda e: e.activation(out=kstat[:, 1:2], in_=kstat[:, 0:1], func=AF.Sqrt, scale=1.0 / 64, bias=EPS),
                     reads=[kstat], writes=[kstat])
                S.op("dve", lambda e: e.reciprocal(out=kstat[:, 2:3], in_=kstat[:, 1:2]), reads=[kstat], writes=[kstat])
                S.op("dve", lambda e: e.scalar_tensor_tensor(out=kin[:], in0=pD[:, 0:64], scalar=kstat[:, 2:3], in1=idxg[:],
                                                             op0=ALU.mult, op1=ALU.mult),
                     reads=[pD, kstat, idxg], writes=[kin])
                S.op("dve", lambda e: e.tensor_scalar(out=wis[p][:], in0=pD[:, 64:72], scalar1=float(8 ** -0.5), scalar2=None,
                                                      op0=ALU.mult), reads=[pD], writes=[wis[p]])
                S.dma("sp", wi_o[tsl, :], wis[p][:], wis[p], load=False)
                emit_rope(S, kin, kin[:], cs[:, 2, :], cs[:, 3, :], tki, kir[p], kir[p][:], 1)
                S.op("pe", lambda e: e.transpose(ptr[0][0:64, 0, :], kir[p][:, 0:64], ident[:]),
                     reads=[kir[p], ident], writes=[ptr[0]])
                S.op("act", lambda e: e.copy(out=kiTs[p][:], in_=ptr[0][0:64, 0, :]), reads=[ptr[0]], writes=[kiTs[p]])
                S.dma("sp", kiT_o[:, tsl], kiTs[p][:], kiTs[p], load=False)
        S.finish()
    return nc


def build_attn0(nu=2, nblk=NBLK, lambda_init=0.2, glob=None):
    nc = _mknc(glob)
    IO = _IO(nc, glob)
    tt = nblk * 128
    QT = IO("QT", [nu, 128, tt], BF16, "ExternalInput")
    KT = IO("KT", [nu, 128, tt], BF16, "ExternalInput")
    V = IO("V", [nu, tt, 129], BF16, "ExternalInput")
    lamv = IO("lamv", [4, 64], F32, "ExternalInput")
    subg = IO("subg", [128], F32, "ExternalInput")
    O = IO("O", [nu, tt, 128], BF16, "ExternalOutput")
    with ExitStack() as es:
        S = Sched(nc, es, glob)
        trif = S.sbuf("trif", [128, 128], F32)
        tri = S.sbuf("tri", [128, 128], BF16)
        S.op("pool", lambda e: e.memset(trif[:], 1.0), writes=[trif])
        S.op("pool", lambda e: e.affine_select(out=trif[:], in_=trif[:], pattern=[[1, 128]], compare_op=ALU.is_ge,
                                               fill=0.0, base=0, channel_multiplier=-1), reads=[trif], writes=[trif])
        S.op("pool", lambda e: e.tensor_copy(out=tri[:], in_=trif[:]), reads=[trif], writes=[tri])
        lv = S.sbuf("lv", [128, 4, 64], F32)
        for i in range(4):
            S.dma("sp", lv[:, i, :], lamv[i, :].partition_broadcast(128), lv, load=True)
        gs = load_bcast(S, "gs", subg, 128)
        S.op("dve", lambda e: e.tensor_scalar(out=gs[:], in0=gs[:], scalar1=float(1.0 - lambda_init), scalar2=None, op0=ALU.mult),
             reads=[gs], writes=[gs])
        lt = S.sbuf("lt", [128, 2, 64], F32)
        ls = S.sbuf("ls", [128, 8], F32)
        S.op("dve", lambda e: e.tensor_tensor(out=lt[:, 0, :], in0=lv[:, 0, :], in1=lv[:, 1, :], op=ALU.mult), reads=[lv], writes=[lt])
        S.op("dve", lambda e: e.tensor_tensor(out=lt[:, 1, :], in0=lv[:, 2, :], in1=lv[:, 3, :], op=ALU.mult), reads=[lv, lt], writes=[lt])
        S.op("dve", lambda e: e.tensor_reduce(out=ls[:, 0:2], in_=lt[:], axis=AX.X, op=ALU.add), reads=[lt], writes=[ls])
        S.op("act", lambda e: e.activation(out=ls[:, 2:4], in_=ls[:, 0:2], func=AF.Exp), reads=[ls], writes=[ls])
        S.op("dve", lambda e: e.tensor_tensor(out=ls[:, 4:5], in0=ls[:, 2:3], in1=ls[:, 3:4], op=ALU.subtract), reads=[ls], writes=[ls])
        S.op("dve", lambda e: e.tensor_scalar(out=ls[:, 5:6], in0=ls[:, 4:5], scalar1=float(lambda_init), scalar2=-1.0,
                                              op0=ALU.add, op1=ALU.mult), reads=[ls], writes=[ls])
        qt_sb = S.sbuf("qt_sb", [128, tt], BF16)
        kt_sb = S.sbuf("kt_sb", [128, tt], BF16)
        v_sb = S.sbuf("v_sb", [128, nblk, 129], BF16)
        ps = [S.psum("ps%d" % i, [128, 512], F32) for i in range(4)]
        acc = [S.psum("acc%d" % i, [128, 512], F32) for i in range(3)]
        pt = [S.sbuf("pt%d" % i, [128, 512], BF16) for i in range(4)]
        fst = [S.sbuf("fst%d" % i, [128, 8], F32) for i in range(2)]
        o0 = [S.sbuf("o0_%d" % i, [128, 128], F32) for i in range(2)]
        o1 = [S.sbuf("o1_%d" % i, [128, 128], F32) for i in range(2)]
        fj = S.sbuf("fj", [128, 128], F32)
        ob = [S.sbuf("ob%d" % i, [128, 128], BF16) for i in range(2)]

        def accv(c, s):
            idx = c * 4 + s
            return acc[idx // 3], acc[idx // 3][:, (idx % 3) * 129:(idx % 3) * 129 + 129]

        fin_cnt = 0
        for u in range(nu):
            nchunk = 4
            csz = (tt + nchunk - 1) // nchunk
            for ci in range(nchunk):
                a, b_ = ci * csz, min(tt, (ci + 1) * csz)
                S.dma("sp", kt_sb[:, a:b_], KT[u, :, a:b_], kt_sb, load=True)
                S.dma("sp", qt_sb[:, a:b_], QT[u, :, a:b_], qt_sb, load=True)
            vsrc = V[u].rearrange("(j p) e -> p j e", p=128)
            for j0 in range(0, nblk, 16):
                j1 = min(nblk, j0 + 16)
                S.dma("sp", v_sb[:, j0:j1, :], vsrc[:, j0:j1, :], v_sb, load=True)
            ntile = (nblk + 3) // 4
            for qt in range(ntile):
                qb0 = qt * 4
                nsub = min(4, nblk - qb0)
                q0 = qb0 * 128
                steps = [(c, j) for j in range(qb0 + nsub) for c in range(2)]
                n = len(steps)
                bank_started = [False, False, False]

                def emitS(i):
                    c, j = steps[i]
                    s0 = max(0, j - qb0)
                    pb = ps[i % 4]
                    S.op("pe", lambda e: e.matmul(pb[:, s0 * 128:nsub * 128],
                                                  lhsT=kt_sb[c * 64:(c + 1) * 64, j * 128:(j + 1) * 128],
                                                  rhs=qt_sb[c * 64:(c + 1) * 64, q0 + s0 * 128:q0 + nsub * 128],
                                                  start=True, stop=True),
                         reads=[kt_sb, qt_sb], writes=[pb])

                def emitE(i):
                    c, j = steps[i]
                    s0 = max(0, j - qb0)
                    pb = ps[i % 4]
                    pp = pt[i % 4]
                    S.op("act", lambda e: e.activation(out=pp[:, s0 * 128:nsub * 128], in_=pb[:, s0 * 128:nsub * 128], func=AF.Exp),
                         reads=[pb], writes=[pp])
                    if j >= qb0:
                        S.op("dve", lambda e: e.tensor_tensor(out=pp[:, s0 * 128:(s0 + 1) * 128], in0=pp[:, s0 * 128:(s0 + 1) * 128],
                                                              in1=tri[:], op=ALU.mult), reads=[pp, tri], writes=[pp])

                def emitPV(i):
                    c, j = steps[i]
                    s0 = max(0, j - qb0)
                    pp = pt[i % 4]
                    fns = []
                    wr = []
                    for s in range(s0, nsub):
                        ab, av = accv(c, s)
                        bi = (c * 4 + s) // 3
                        st = not bank_started[bi]
                        bank_started[bi] = True
                        if ab not in wr:
                            wr.append(ab)
                        fns.append(lambda e, s=s, av=av, st=st: e.matmul(av, lhsT=pp[:, s * 128:(s + 1) * 128], rhs=v_sb[:, j, :],
                                                                         start=st, stop=(j == qb0 + s), skip_group_check=True))
                    S.multi("pe", fns, reads=[pp, v_sb], writes=wr)

                for i in range(min(2, n)):
                    emitS(i)
                for i in range(0, n, 2):
                    for k in (2, 3):
                        if i + k < n:
                            emitS(i + k)
                    for k in (0, 1):
                        if i + k < n:
                            emitE(i + k)
                            emitPV(i + k)
                for s in range(nsub):
                    f = fin_cnt % 2
                    fin_cnt += 1
                    a0b, a0 = accv(0, s)
                    a1b, a1 = accv(1, s)
                    st_ = fst[f]
                    S.op("dve", lambda e: e.reciprocal(out=st_[:, 0:1], in_=a0[:, 128:129]), reads=[a0b], writes=[st_])
                    S.op("dve", lambda e: e.reciprocal(out=st_[:, 1:2], in_=a1[:, 128:129]), reads=[a1b, st_], writes=[st_])
                    S.op("dve", lambda e: e.tensor_tensor(out=st_[:, 2:3], in0=st_[:, 1:2], in1=ls[:, 5:6], op=ALU.mult),
                         reads=[st_, ls], writes=[st_])
                    S.op("dve", lambda e: e.tensor_scalar(out=o0[f][:], in0=a0[:, 0:128], scalar1=st_[:, 0:1], scalar2=None, op0=ALU.mult),
                         reads=[a0b, st_], writes=[o0[f]])
                    S.op("dve", lambda e: e.scalar_tensor_tensor(out=o1[f][:], in0=a1[:, 0:128], scalar=st_[:, 2:3], in1=o0[f][:],
                                                                 op0=ALU.mult, op1=ALU.add), reads=[a1b, st_, o0[f]], writes=[o1[f]])
                    S.op("act", lambda e: e.activation(out=fj[:], in_=o1[f][:], func=AF.Square, accum_out=st_[:, 3:4]),
                         reads=[o1[f], st_], writes=[fj, st_])
                    S.op("act", lambda e: e.activation(out=st_[:, 4:5], in_=st_[:, 3:4], func=AF.Sqrt, scale=1.0 / 128, bias=EPS),
                         reads=[st_], writes=[st_])
                    S.op("dve", lambda e: e.reciprocal(out=st_[:, 5:6], in_=st_[:, 4:5]), reads=[st_], writes=[st_])
                    S.op("dve", lambda e: e.scalar_tensor_tensor(out=ob[f][:], in0=o1[f][:], scalar=st_[:, 5:6], in1=gs[:],
                                                                 op0=ALU.mult, op1=ALU.mult), reads=[o1[f], st_, gs], writes=[ob[f]])
                    S.dma("sp", O[u, q0 + s * 128:q0 + (s + 1) * 128, :], ob[f][:], ob[f], load=False)
        S.finish()
    return nc


def build_ffnp(final, nblk=CB, glob=None):
    nc = _mknc(glob)
    IO = _IO(nc, glob)
    ct = nblk * 128
    h_in = IO("h_in", [ct, D], F32, "ExternalInput")
    o_in = IO("o_in", [ct, D], BF16, "ExternalInput")
    wo_in = IO("wo_in", [D, D], F32, "ExternalInput")
    g_in = IO("g_in", [D], F32, "ExternalInput")
    wup_in = IO("wup_in", [D, 2 * FF], F32, "ExternalInput")
    cpar_in = IO("cpar_in", [128, 2 * NFC, 4], F32, "ExternalInput")
    wdn_in = IO("wdn_in", [FF, D], F32, "ExternalInput")
    if final:
        gf_in = IO("gf_in", [D], F32, "ExternalInput")
    h_out = IO("h_out", [ct, D], F32, "ExternalOutput")
    with ExitStack() as es:
        S = Sched(nc, es, glob)
        ident = make_ident(S, nc)
        g_bc = load_bcast(S, "g_bc", g_in, D)
        if final:
            gf_bc = load_bcast(S, "gf_bc", gf_in, D)
        cpar = S.sbuf("cpar", [128, 2 * NFC, 4], F32)
        S.dma("sp", cpar[:], cpar_in, cpar, load=True)
        wo = load_weight_bf16(S, "wo", wo_in, D)
        hres = S.sbuf("hres", [128, 4, D], F32)
        hnT = S.sbuf("hnT", [128, 8, 512], BF16)
        aT = S.sbuf("aT", [128, NFC, 512], BF16)
        wdn = S.sbuf("wdn", [128, NFC, D], BF16)
        wu = [S.sbuf("wu%d" % i, [128, 8, 2, 128], BF16) for i in range(4)]
        ob = [S.sbuf("ob%d" % i, [128, D], BF16) for i in range(2)]
        oT = [S.sbuf("oT%d" % i, [128, 8, 128], BF16) for i in range(2)]
        hn = S.sbuf("hn", [128, D], BF16)
        junk = S.sbuf("junk", [128, D], BF16)
        stat = S.sbuf("stat", [128, 8], F32)
        ub = [S.sbuf("ub%d" % i, [128, 514], F32) for i in range(4)]
        cg = [S.sbuf("cg%d" % i, [128, 512], F32) for i in range(4)]
        sg = [S.sbuf("sg%d" % i, [128, 512], F32) for i in range(2)]
        carry = S.sbuf("carry", [128, 2 * NFC, 2], F32)
        if final:
            fo = [S.sbuf("fo%d" % i, [128, D], F32) for i in range(2)]
        ptr = S.psum("ptr", [128, 8, 128], BF16)
        pY = [S.psum("pY%d" % i, [128, 512], F32) for i in range(2)]
        pU = [S.psum("pU%d" % i, [128, 512], F32) for i in range(4)]
        S.op("dve", lambda e: e.memset(carry[:], 0.0), writes=[carry])
        wup_v = wup_in.rearrange("(k p) (t f) -> p k t f", p=128, t=2)
        wdn_v = wdn_in.rearrange("(c p) n -> p c n", p=128)
        wup_s = nc.dram_tensor(S.pfx + "wup_s", [NFC, 128, 2048], BF16, kind="Internal").ap()
        wdn_s = nc.dram_tensor(S.pfx + "wdn_s", [128, NFC, D], BF16, kind="Internal").ap()
        wup_sb = Buf(None, "wup_scr")
        wdn_sb = Buf(None, "wdn_scr")
        for c0 in range(0, NFC, 6):
            c1 = min(NFC, c0 + 6)
            S.dma("pool", wdn_s[:, c0:c1, :], wdn_v[:, c0:c1, :], wdn_sb, load=True)
        for c in range(NFC):
            dstv = wup_s[c].rearrange("p (k t f) -> p k t f", k=8, t=2)
            for t in range(2):
                S.dma("pool", dstv[:, :, t, :], wup_v[:, :, t, c * 128:(c + 1) * 128], wup_sb, load=True)
        tiles = [(b0, min(4, nblk - b0)) for b0 in range(0, nblk, 4)]
        ycnt = 0
        for (b0, nb) in tiles:
            ntok = nb * 128
            for c0 in range(0, NFC, 11):
                c1 = min(NFC, c0 + 11)
                S.dma("sp", wdn[:, c0:c1, :], wdn_s[:, c0:c1, :], wdn, load=True, extra_reads=[wdn_sb])
            for s in range(nb):
                blk = b0 + s
                tsl = slice(blk * 128, (blk + 1) * 128)
                p = s % 2
                S.dma("sp", hres[:, s, :], h_in[tsl, :], hres, load=True)
            S.dma("sp", ob[0][:], o_in[b0 * 128:(b0 + 1) * 128, :], ob[0], load=True)
            for s in range(nb):
                p = s % 2
                if s + 1 < nb:
                    S.dma("sp", ob[1 - p][:], o_in[(b0 + s + 1) * 128:(b0 + s + 2) * 128, :], ob[1 - p], load=True)
                S.multi("pe", [(lambda e, k=k: e.transpose(ptr[:, k, :], ob[p][:, k * 128:(k + 1) * 128], ident[:])) for k in range(8)],
                        reads=[ob[p], ident], writes=[ptr])
                S.op("act", lambda e: e.copy(out=oT[p][:], in_=ptr[:]), reads=[ptr], writes=[oT[p]])
                for hf in range(2):
                    py = pY[ycnt % 2]
                    ycnt += 1
                    S.multi("pe", [(lambda e, k=k: e.matmul(py[:], lhsT=oT[p][:, k, :], rhs=wo[:, k, hf * 512:(hf + 1) * 512],
                                                            start=(k == 0), stop=(k == 7))) for k in range(8)],
                            reads=[oT[p], wo], writes=[py])
                    S.op("dve", lambda e: e.tensor_tensor(out=hres[:, s, hf * 512:(hf + 1) * 512], in0=py[:],
                                                          in1=hres[:, s, hf * 512:(hf + 1) * 512], op=ALU.add),
                         reads=[py, hres], writes=[hres])
            for s in range(nb):
                emit_rmsnorm_T(S, hres, hres[:, s, :], g_bc, hn, stat, junk, ptr, ident, hnT, hnT[:, :, s * 128:(s + 1) * 128])
            for c in range(NFC):
                w = wu[c % 4]
                S.dma("sp", w[:].rearrange("p k t f -> p (k t f)"), wup_s[c], w, load=True, extra_reads=[wup_sb])
                pg = pU[(2 * c) % 4]
                pv = pU[(2 * c + 1) % 4]
                for (pp, t) in ((pg, 0), (pv, 1)):
                    S.multi("pe", [(lambda e, k=k: e.matmul(pp[:, 0:ntok], lhsT=w[:, k, t, :], rhs=hnT[:, k, 0:ntok],
                                                            start=(k == 0), stop=(k == 7))) for k in range(8)],
                            reads=[w, hnT], writes=[pp])
                cvals = []
                for (pp, t) in ((pg, 0), (pv, 1)):
                    cc = t * NFC + c
                    u_ = ub[(2 * c + t) % 4]
                    cgb = cg[(2 * c + t) % 4]
                    S.op("act", lambda e: e.copy(out=u_[:, 2:2 + ntok], in_=pp[:, 0:ntok]), reads=[pp], writes=[u_])
                    S.op("dve", lambda e: e.tensor_copy(out=u_[:, 0:2], in_=carry[:, cc, :]), reads=[carry, u_], writes=[u_])
                    S.op("dve", lambda e: e.tensor_copy(out=carry[:, cc, :], in_=u_[:, ntok:ntok + 2]), reads=[u_, carry], writes=[carry])
                    S.op("pool", lambda e: e.tensor_scalar(out=cgb[:, 0:ntok], in0=u_[:, 2:2 + ntok], scalar1=cpar[:, cc, 2:3],
                                                           scalar2=cpar[:, cc, 3:4], op0=ALU.mult, op1=ALU.add),
                         reads=[u_, cpar], writes=[cgb])
                    S.op("dve", lambda e: e.scalar_tensor_tensor(out=cgb[:, 0:ntok], in0=u_[:, 1:1 + ntok], scalar=cpar[:, cc, 1:2],
                                                                 in1=cgb[:, 0:ntok], op0=ALU.mult, op1=ALU.add),
                         reads=[u_, cpar, cgb], writes=[cgb])
                    S.op("dve", lambda e: e.scalar_tensor_tensor(out=cgb[:, 0:ntok], in0=u_[:, 0:ntok], scalar=cpar[:, cc, 0:1],
                                                                 in1=cgb[:, 0:ntok], op0=ALU.mult, op1=ALU.add),
                         reads=[u_, cpar, cgb], writes=[cgb])
                    cvals.append(cgb)
                sgb = sg[c % 2]
                S.op("act", lambda e: e.activation(out=sgb[:, 0:ntok], in_=cvals[0][:, 0:ntok], func=AF.Silu),
                     reads=[cvals[0]], writes=[sgb])
                S.op("pool", lambda e: e.tensor_tensor(out=aT[:, c, 0:ntok], in0=sgb[:, 0:ntok], in1=cvals[1][:, 0:ntok], op=ALU.mult),
                     reads=[sgb, cvals[1]], writes=[aT])
            for s in range(nb):
                for hf in range(2):
                    py = pY[ycnt % 2]
                    ycnt += 1
                    S.multi("pe", [(lambda e, c=c: e.matmul(py[:], lhsT=aT[:, c, s * 128:(s + 1) * 128],
                                                            rhs=wdn[:, c, hf * 512:(hf + 1) * 512],
                                                            start=(c == 0), stop=(c == NFC - 1))) for c in range(NFC)],
                            reads=[aT, wdn], writes=[py])
                    S.op("dve", lambda e: e.tensor_tensor(out=hres[:, s, hf * 512:(hf + 1) * 512], in0=py[:],
                                                          in1=hres[:, s, hf * 512:(hf + 1) * 512], op=ALU.add),
                         reads=[py, hres], writes=[hres])
            for s in range(nb):
                blk = b0 + s
                tsl = slice(blk * 128, (blk + 1) * 128)
                if final:
                    f = fo[s % 2]
                    S.op("act", lambda e: e.activation(out=junk[:], in_=hres[:, s, :], func=AF.Square, accum_out=stat[:, 0:1]),
                         reads=[hres], writes=[junk, stat])
                    S.op("act", lambda e: e.activation(out=stat[:, 1:2], in_=stat[:, 0:1], func=AF.Sqrt, scale=1.0 / D, bias=EPS),
                         reads=[stat], writes=[stat])
                    S.op("dve", lambda e: e.reciprocal(out=stat[:, 2:3], in_=stat[:, 1:2]), reads=[stat], writes=[stat])
                    S.op("dve", lambda e: e.scalar_tensor_tensor(out=f[:], in0=hres[:, s, :], scalar=stat[:, 2:3], in1=gf_bc[:],
                                                                 op0=ALU.mult, op1=ALU.mult), reads=[hres, stat, gf_bc], writes=[f])
                    S.dma("sp", h_out[tsl, :], f[:], f, load=False)
                else:
                    S.dma("sp", h_out[tsl, :], hres[:, s, :], hres, load=False)
        S.finish()
    return nc


NIT = 33
NKB = 132


def build_dsa_mask(nit=NIT, glob=None, fused_nblk=None):
    nc = _mknc(glob)
    if glob is None:
        nkb = 4 * nit
        tot = 2 * nit * (nit + 1)
        kiT2 = nc.dram_tensor("kiT2", [128, nkb * 128], BF16, kind="ExternalInput").ap()
        qiT = nc.dram_tensor("qiT", [nit, 128, 4, 128], BF16, kind="ExternalInput").ap()
        wi = nc.dram_tensor("wi", [nit, 128, 8], F32, kind="ExternalInput").ap()
        cmask = nc.dram_tensor("cmask", [128, 512], F32, kind="ExternalInput").ap()
        maskT = nc.dram_tensor("maskT", [128, tot, 128], BF16, kind="ExternalOutput").ap()
        items = [(m, 0, qiT[m], wi[m], 2 * m * (m + 1), 4 * m + 4, maskT) for m in range(nit)]
        ncm = 1
    else:
        nblk_f = fused_nblk
        nkb = 4 * ((nblk_f + 3) // 4)
        kiT_i, qiT_i, wi_i = glob.io["kiT"], glob.io["qiT"], glob.io["wi"]
        cmask4 = glob.io["cmask4"]
        items = [(i // 4, i % 4, qiT_i[:, :, i * 128:(i + 1) * 128].rearrange("c p t -> p c t"), wi_i[i * 128:(i + 1) * 128, :],
                  glob.io["MT"](i)[1], i + 1, glob.io["MT"](i)[0]) for i in range(nblk_f)]
        ncm = 4
    with ExitStack() as es:
        S = Sched(nc, es, glob)
        identf = S.sbuf("identf", [128, 128], F32)
        ident = S.sbuf("ident", [128, 128], BF16)
        S.op("pool", lambda e: e.memset(identf[:], 0.0), writes=[identf])
        S.op("pool", lambda e: e.affine_select(out=identf[:], in_=identf[:], pattern=[[-1, 128]], compare_op=ALU.not_equal,
                                               fill=1.0, base=0, channel_multiplier=1), reads=[identf], writes=[identf])
        S.op("pool", lambda e: e.tensor_copy(out=ident[:], in_=identf[:]), reads=[identf], writes=[ident])
        ki = S.sbuf("ki", [128, nkb * 128], BF16)
        cm = S.sbuf("cm", [128, ncm, 512], F32)
        if glob is None:
            for a in range(0, nkb * 128, 4224):
                b_ = min(nkb * 128, a + 4224)
                S.dma("sp", ki[:, a:b_], kiT2[:, a:b_], ki, load=True)
            S.dma("sp", cm[:, 0, :], cmask, cm, load=True)
        else:
            tt_f = nblk_f * 128
            if nkb * 128 > tt_f:
                S.op("pool", lambda e: e.memset(ki[:, tt_f:nkb * 128], 0.0), writes=[ki])
            for a in range(0, tt_f, 4224):
                b_ = min(tt_f, a + 4224)
                S.dma("sp", ki[0:64, a:b_], kiT_i[:, a:b_], ki, load=True)
                S.dma("sp", ki[64:128, a:b_], kiT_i[:, a:b_], ki, load=True)
            S.dma("sp", cm[:], cmask4.rearrange("c p k -> p c k"), cm, load=True)
        half = S.sbuf("half", [128, 1], F32)
        S.op("dve", lambda e: e.memset(half[:], 0.5), writes=[half])
        sc = S.sbuf("sc", [128, nkb * 128], F32)
        m01 = S.sbuf("m01", [128, nkb * 128], BF16)
        aj = S.sbuf("aj", [128, nkb * 64 + 128], BF16)
        qb = [S.sbuf("qb%d" % i, [128, 4, 128], BF16) for i in range(2)]
        wb = [S.sbuf("wb%d" % i, [128, 8], F32) for i in range(2)]
        dg = [S.sbuf("dg%d" % i, [128, 8, 128], BF16) for i in range(2)]
        rl = [S.sbuf("rl%d" % i, [128, 512], BF16) for i in range(4)]
        pI = [S.psum("pI%d" % i, [128, 512], F32) for i in range(4)]
        pA = [S.psum("pA%d" % i, [128, 512], F32) for i in range(2)]
        ptr = [S.psum("ptr%d" % i, [128, 8, 128], BF16) for i in range(2)]
        stg = [S.sbuf("stg%d" % i, [128, 8, 128], BF16) for i in range(2)]
        bs = S.sbuf("bs", [128, 16], F32)
        nm = S.sbuf("nm", [128, 1], F32)
        as_ = S.sbuf("as_", [128, 1], F32)
        step = 0
        tcnt = 0
        gcnt = 0
        for it_idx, (m, cp, qi_ap, wi_ap, off, nout, maskT) in enumerate(items):
            p = it_idx % 2
            nk = 4 * m + 4
            tc_ = nk * 128
            S.dma("sp", qb[p][:], qi_ap, qb[p], load=True)
            S.dma("sp", wb[p][:], wi_ap, wb[p], load=True)
            S.op("dve", lambda e: e.tensor_tensor(out=dg[p][:], in0=identf[:].unsqueeze(1).broadcast_to([128, 8, 128]),
                                                  in1=wb[p][:].unsqueeze(2).broadcast_to([128, 8, 128]), op=ALU.mult),
                 reads=[identf, wb[p]], writes=[dg[p]])
            seq = [(g, h) for g in range(m + 1) for h in range(8)]
            n = len(seq)

            def emitR(i):
                g, h = seq[i]
                pi = pI[(step + i) % 4]
                pr = slice((h % 2) * 64, (h % 2) * 64 + 64)
                S.op("pe", lambda e: e.matmul(pi[:], lhsT=qb[p][pr, h // 2, :], rhs=ki[pr, g * 512:(g + 1) * 512], start=True, stop=True),
                     reads=[qb[p], ki], writes=[pi])

            def emitA(i):
                g, h = seq[i]
                pi = pI[(step + i) % 4]
                r_ = rl[(step + i) % 4]
                pa = pA[(gcnt + g) % 2]
                S.op("act", lambda e: e.activation(out=r_[:], in_=pi[:], func=AF.Relu), reads=[pi], writes=[r_])
                S.op("pe", lambda e: e.matmul(pa[:], lhsT=dg[p][:, h, :], rhs=r_[:], start=(h == 0), stop=(h == 7)),
                     reads=[dg[p], r_], writes=[pa])
                if h == 7:
                    S.op("act", lambda e: e.copy(out=sc[:, g * 512:(g + 1) * 512], in_=pa[:]), reads=[pa], writes=[sc])

            for i in range(min(2, n)):
                emitR(i)
            for i in range(0, n, 2):
                for k in (2, 3):
                    if i + k < n:
                        emitR(i + k)
                for k in (0, 1):
                    if i + k < n:
                        emitA(i + k)
            step += n
            gcnt += m + 1
            S.op("dve", lambda e: e.tensor_reduce(out=bs[:, 0:1], in_=sc[:, 16:tc_], axis=AX.X, op=ALU.min), reads=[sc], writes=[bs])
            S.op("dve", lambda e: e.tensor_reduce(out=bs[:, 1:2], in_=sc[:, 16:tc_], axis=AX.X, op=ALU.max), reads=[sc, bs], writes=[bs])
            S.op("dve", lambda e: e.tensor_scalar(out=bs[:, 0:1], in0=bs[:, 0:1], scalar1=-1.0, scalar2=None, op0=ALU.add), reads=[bs], writes=[bs])
            S.op("dve", lambda e: e.tensor_scalar(out=bs[:, 1:2], in0=bs[:, 1:2], scalar1=1.0, scalar2=None, op0=ALU.add), reads=[bs], writes=[bs])
            S.op("dve", lambda e: e.memset(sc[:, 0:16], BIGM), reads=[sc], writes=[sc])
            S.op("dve", lambda e: e.tensor_tensor(out=sc[:, tc_ - 512:tc_], in0=sc[:, tc_ - 512:tc_], in1=cm[:, cp, :], op=ALU.add),
                 reads=[sc, cm], writes=[sc])
            hf_ = (nk // 2) * 128
            nact = tc_ - hf_
            thr_c = float(TOPK) - 0.5 - 0.5 * nact
            for it in range(N_BISECT):
                S.op("dve", lambda e: e.scalar_tensor_tensor(out=bs[:, 2:3], in0=bs[:, 0:1], scalar=bs[:, 1:2], in1=half[:],
                                                             op0=ALU.add, op1=ALU.mult), reads=[bs, half], writes=[bs])
                S.op("dve", lambda e: e.tensor_scalar(out=nm[:], in0=bs[:, 2:3], scalar1=-1.0, scalar2=None, op0=ALU.mult),
                     reads=[bs], writes=[nm])
                S.op("act", lambda e: e.activation(out=aj[:, 0:nact], in_=sc[:, hf_:tc_], func=AF.Sign, bias=nm[:], scale=1.0,
                                                   accum_out=as_[:]), reads=[sc, nm], writes=[aj, as_])
                S.op("dve", lambda e: e.tensor_scalar(out=m01[:, 0:hf_], in0=sc[:, 0:hf_], scalar1=bs[:, 2:3], scalar2=None,
                                                      op0=ALU.is_ge, op1=ALU.add, accum_out=bs[:, 3:4]),
                     reads=[sc, bs], writes=[m01, bs])
                S.op("dve", lambda e: e.scalar_tensor_tensor(out=bs[:, 8:9], in0=as_[:], scalar=half[:], in1=bs[:, 3:4],
                                                             op0=ALU.mult, op1=ALU.add), reads=[as_, half, bs], writes=[bs])
                S.op("dve", lambda e: e.tensor_scalar(out=bs[:, 4:5], in0=bs[:, 8:9], scalar1=thr_c, scalar2=None, op0=ALU.is_ge),
                     reads=[bs], writes=[bs])
                S.op("dve", lambda e: e.tensor_tensor(out=bs[:, 5:6], in0=bs[:, 2:3], in1=bs[:, 0:1], op=ALU.subtract), reads=[bs], writes=[bs])
                S.op("dve", lambda e: e.tensor_tensor(out=bs[:, 6:7], in0=bs[:, 1:2], in1=bs[:, 2:3], op=ALU.subtract), reads=[bs], writes=[bs])
                S.op("dve", lambda e: e.scalar_tensor_tensor(out=bs[:, 0:1], in0=bs[:, 5:6], scalar=bs[:, 4:5], in1=bs[:, 0:1],
                                                             op0=ALU.mult, op1=ALU.add), reads=[bs], writes=[bs])
                S.op("dve", lambda e: e.scalar_tensor_tensor(out=bs[:, 1:2], in0=bs[:, 6:7], scalar=bs[:, 4:5], in1=bs[:, 2:3],
                                                             op0=ALU.mult, op1=ALU.add), reads=[bs], writes=[bs])
            S.op("dve", lambda e: e.tensor_scalar(out=m01[:, 0:tc_], in0=sc[:, 0:tc_], scalar1=bs[:, 0:1], scalar2=None, op0=ALU.is_ge),
                 reads=[sc, bs, m01], writes=[m01])
            for j0 in range(0, nout, 8):
                nb_ = min(8, nout - j0)
                pt_ = ptr[tcnt % 2]
                sg_ = stg[tcnt % 2]
                tcnt += 1
                S.multi("pe", [(lambda e, j=j: e.transpose(pt_[:, j, :], m01[:, (j0 + j) * 128:(j0 + j + 1) * 128], ident[:]))
                               for j in range(nb_)], reads=[m01, ident], writes=[pt_])
                S.op("act", lambda e: e.copy(out=sg_[:, 0:nb_, :], in_=pt_[:, 0:nb_, :]), reads=[pt_], writes=[sg_])
                S.dma("sp", maskT[:, off + j0:off + j0 + nb_, :], sg_[:, 0:nb_, :], sg_, load=False)
        S.finish()
    return nc


def build_dsa_attn(nblk=NBLK, glob=None):
    nc = _mknc(glob)
    IO = _IO(nc, glob)
    tt = nblk * 128
    tot = nblk * (nblk + 1) // 2
    QT = IO("QT", [128, 2, tt], BF16, "ExternalInput")
    KT = IO("KT", [128, 2, tt], BF16, "ExternalInput")
    V = IO("V", [tt, 4, 65], BF16, "ExternalInput")
    if glob is None:
        MT_ = IO("MT", [128, tot, 128], BF16, "ExternalInput")
        mt_sel = lambda i: (MT_, i * (i + 1) // 2)
    else:
        mt_sel = glob.io["MT"]
    O = IO("O", [tt, 4, 64], BF16, "ExternalOutput")
    with ExitStack() as es:
        S = Sched(nc, es, glob)
        kt = S.sbuf("kt", [128, 2, tt], BF16)
        for c in range(2):
            for a in range(0, tt, 4224):
                b_ = min(tt, a + 4224)
                S.dma("sp", kt[:, c, a:b_], KT[:, c, a:b_], kt, load=True)
        v_sb = S.sbuf("v_sb", [128, nblk, 260], BF16)
        vsrc = V.rearrange("(j p) h e -> p j (h e)", p=128)
        for j0 in range(0, nblk, 16):
            j1 = min(nblk, j0 + 16)
            S.dma("sp", v_sb[:, j0:j1, :], vsrc[:, j0:j1, :], v_sb, load=True)
        qb = [S.sbuf("qb%d" % i, [128, 2, 128], BF16) for i in range(2)]
        mp = [S.sbuf("mp%d" % i, [128, 16, 128], BF16) for i in range(3)]
        pS = [S.psum("pS%d" % i, [128, 512], F32) for i in range(4)]
        acc = [S.psum("acc%d" % i, [128, 512], F32) for i in range(2)]
        pt = [S.sbuf("pt%d" % i, [128, 512], BF16) for i in range(4)]
        r4 = [S.sbuf("r4_%d" % i, [128, 4], F32) for i in range(2)]
        ob = [S.sbuf("ob%d" % i, [128, 4, 64], BF16) for i in range(2)]
        mcnt = 0
        gstep = 0
        for i in range(nblk):
            p = i % 2
            nk = i + 1
            MT, off = mt_sel(i)
            S.dma("sp", qb[p][:], QT[:, :, i * 128:(i + 1) * 128], qb[p], load=True)
            ab = acc[p]
            steps = []
            pieces = []
            for pc0 in range(0, nk, 16):
                pc1 = min(nk, pc0 + 16)
                pieces.append((pc0, pc1))
            started = [False]

            def load_piece(pi_):
                pc0, pc1 = pieces[pi_]
                mb = mp[(mcnt + pi_) % 3]
                S.dma("sp", mb[:, 0:pc1 - pc0, :], MT[:, off + pc0:off + pc1, :], mb, load=True)

            for pi_, (pc0, pc1) in enumerate(pieces):
                for hp in range(2):
                    for g0 in range(pc0, pc1, 4):
                        steps.append((pi_, hp, g0, min(pc1, g0 + 4)))
            n = len(steps)

            def emitS(si):
                pi_, hp, g0, g1 = steps[si]
                base = (2 * (gstep + si)) % 4
                fns = []
                for jj, blk in enumerate(range(g0, g1)):
                    for hl in range(2):
                        pr = slice(hl * 64, hl * 64 + 64)
                        pb = pS[base + hl]
                        fns.append(lambda e, jj=jj, blk=blk, pr=pr, pb=pb: e.matmul(
                            pb[:, jj * 128:(jj + 1) * 128], lhsT=kt[pr, hp, blk * 128:(blk + 1) * 128],
                            rhs=qb[p][pr, hp, :], start=True, stop=True))
                S.multi("pe", fns, reads=[kt, qb[p]], writes=[pS[base], pS[base + 1]])

            def emitE(si):
                pi_, hp, g0, g1 = steps[si]
                pc0, pc1 = pieces[pi_]
                w_ = (g1 - g0) * 128
                base = (2 * (gstep + si)) % 4
                mb = mp[(mcnt + pi_) % 3]
                for hl in range(2):
                    pb = pS[base + hl]
                    pp = pt[base + hl]
                    S.op("act", lambda e: e.activation(out=pp[:, 0:w_], in_=pb[:, 0:w_], func=AF.Exp), reads=[pb], writes=[pp])
                    S.op("dve", lambda e: e.tensor_tensor(out=pp[:, 0:w_], in0=pp[:, 0:w_],
                                                          in1=mb[:, g0 - pc0:g1 - pc0, :].rearrange("p j q -> p (j q)"), op=ALU.mult),
                         reads=[pp, mb], writes=[pp])

            def emitPV(si):
                pi_, hp, g0, g1 = steps[si]
                base = (2 * (gstep + si)) % 4
                for hl in range(2):
                    hh = 2 * hp + hl
                    pp = pt[base + hl]
                    fns = []
                    for jj, blk in enumerate(range(g0, g1)):
                        st = not started[0]
                        started[0] = True
                        fns.append(lambda e, jj=jj, blk=blk, st=st: e.matmul(ab[:, hh * 65:(hh + 1) * 65], lhsT=pp[:, jj * 128:(jj + 1) * 128],
                                                                             rhs=v_sb[:, blk, hh * 65:(hh + 1) * 65],
                                                                             start=st, stop=(blk == nk - 1), skip_group_check=True))
                    S.multi("pe", fns, reads=[pp, v_sb], writes=[ab])

            LA = 1
            loaded = set()

            def need_piece(si):
                pi_ = steps[si][0]
                if pi_ not in loaded:
                    loaded.add(pi_)
                    load_piece(pi_)

            for si in range(min(LA, n)):
                need_piece(si)
                emitS(si)
            for si in range(n):
                if si + LA < n:
                    need_piece(si + LA)
                    emitS(si + LA)
                nxt = steps[si][0] + 1
                if nxt < len(pieces) and nxt not in loaded:
                    loaded.add(nxt)
                    load_piece(nxt)
                emitE(si)
                emitPV(si)
            gstep += n
            mcnt += len(pieces)
            S.op("dve", lambda e: e.reciprocal(out=r4[p][:], in_=ab[:, 0:260].rearrange("p (h e) -> p h e", e=65)[:, :, 64]),
                 reads=[ab], writes=[r4[p]])
            S.op("dve", lambda e: e.tensor_tensor(out=ob[p][:], in0=ab[:, 0:260].rearrange("p (h e) -> p h e", e=65)[:, :, 0:64],
                                                  in1=r4[p][:].unsqueeze(2).broadcast_to([128, 4, 64]), op=ALU.mult),
                 reads=[ab, r4[p]], writes=[ob[p]])
            S.dma("sp", O[i * 128:(i + 1) * 128, :, :], ob[p][:], ob[p], load=False)
        S.finish()
    return nc


_PROG = {}


def _prog(name, fn):
    if name not in _PROG:
        _PROG[name] = fn()
    return _PROG[name]


def _run(nc, in_maps):
    res = run_bass_kernel_spmd(nc, in_maps, core_ids=list(range(NCORE)))
    return res.results


def _rope_tables():
    inv = (np.float32(10000.0) ** (-(np.arange(0, 64, 2, dtype=np.float32)) / np.float32(64))).astype(np.float32)
    ang = (np.arange(T, dtype=np.float32)[:, None] * inv[None, :]).astype(np.float32)
    cos = np.cos(ang).astype(np.float32)
    sin = np.sin(ang).astype(np.float32)
    return np.stack([cos * np.float32(0.125), sin * np.float32(0.125), cos, sin], axis=1)


def _shard_rows(full, b, c):
    return np.ascontiguousarray(full[b, 32 * c * 128:(32 * c + CB) * 128])


def _cs_shard(cs_full, c):
    s = cs_full[32 * c * 128:(32 * c + CB) * 128]
    return np.ascontiguousarray(s.reshape(CB, 128, 4, 32).transpose(1, 0, 2, 3))


def _assemble_rows(shards, width_shape, dtype):
    out = np.zeros((B, T) + tuple(width_shape), dtype=dtype)
    for r in range(NCORE):
        b, c = divmod(r, 4)
        if c == 0:
            out[b, 0:CT] = shards[r]
        else:
            out[b, (32 * c + 1) * 128:(32 * c + CB) * 128] = shards[r][128:]
    return out


def _assemble_featmajor(shards, nch, prows=128):
    out = np.zeros((B, nch, prows, T), dtype=shards[0].dtype)
    for r in range(NCORE):
        b, c = divmod(r, 4)
        if c == 0:
            out[b, :, :, 0:CT] = shards[r]
        else:
            out[b, :, :, (32 * c + 1) * 128:(32 * c + CB) * 128] = shards[r][:, :, 128:]
    return out


def _cpar(conv_w, conv_b):
    c = np.concatenate([conv_w, conv_b[None, :]], axis=0)
    return np.ascontiguousarray(c.reshape(4, 2 * NFC, 128).transpose(2, 1, 0))


def kernel_unfused(x, meta_tokens, da_norm, da_w_qkv, da_lambda_q1, da_lambda_k1, da_lambda_q2, da_lambda_k2,
           da_subln, da_w_o, dsa_norm, dsa_w_in, dsa_idx_k_norm, dsa_w_o, ffn_norm, ffn_w_up,
           ffn_conv_w, ffn_conv_b, ffn_w_down, final_norm):
    f32 = np.float32
    x = np.asarray(x, f32)
    h0 = np.zeros((B, T, D), f32)
    h0[:, :NMETA] = np.asarray(meta_tokens, f32)[None]
    h0[:, NMETA:NMETA + L] = x
    cs_full = _rope_tables()
    A = lambda a: np.ascontiguousarray(np.asarray(a, f32))

    nc = _prog("projA", lambda: build_proj("A"))
    maps = []
    for r in range(NCORE):
        b, c = divmod(r, 4)
        maps.append({"h_in": _shard_rows(h0, b, c), "g_in": A(da_norm[0]), "w_in": A(da_w_qkv[0]), "cs_in": _cs_shard(cs_full, c)})
    res = _run(nc, maps)
    QT = _assemble_featmajor([r_["qT_o"] for r_ in res], 8)
    KT = _assemble_featmajor([r_["kT_o"] for r_ in res], 8)
    Vf = _assemble_rows([r_["v_o"] for r_ in res], (8, 129), NPBF)
    del res

    nc = _prog("attn0", lambda: build_attn0(2, NBLK, 0.2))
    lamv = np.stack([A(da_lambda_q1[0]), A(da_lambda_k1[0]), A(da_lambda_q2[0]), A(da_lambda_k2[0])])
    maps = []
    for r in range(NCORE):
        us = [divmod(2 * r + k, 8) for k in range(2)]
        maps.append({"QT": np.ascontiguousarray(np.stack([QT[b, h] for b, h in us])),
                     "KT": np.ascontiguousarray(np.stack([KT[b, h] for b, h in us])),
                     "V": np.ascontiguousarray(np.stack([Vf[b, :, h, :] for b, h in us])),
                     "lamv": lamv, "subg": A(da_subln[0])})
    res = _run(nc, maps)
    o1 = np.zeros((B, T, D), NPBF)
    for r in range(NCORE):
        for k in range(2):
            b, h = divmod(2 * r + k, 8)
            o1[b, :, h * 128:(h + 1) * 128] = res[r]["O"][k]
    del res, QT, KT, Vf

    nc = _prog("ffn0", lambda: build_ffnp(False))
    maps = []
    for r in range(NCORE):
        b, c = divmod(r, 4)
        maps.append({"h_in": _shard_rows(h0, b, c), "o_in": _shard_rows(o1, b, c), "wo_in": A(da_w_o[0]), "g_in": A(ffn_norm[0]),
                     "wup_in": A(ffn_w_up[0]), "cpar_in": _cpar(A(ffn_conv_w[0]), A(ffn_conv_b[0])), "wdn_in": A(ffn_w_down[0])})
    res = _run(nc, maps)
    h2 = _assemble_rows([r_["h_out"] for r_ in res], (D,), f32)
    del res, o1, h0

    nc = _prog("projB", lambda: build_proj("B"))
    maps = []
    for r in range(NCORE):
        b, c = divmod(r, 4)
        maps.append({"h_in": _shard_rows(h2, b, c), "g_in": A(dsa_norm[0]), "w_in": A(dsa_w_in[0]), "cs_in": _cs_shard(cs_full, c),
                     "idxg_in": A(dsa_idx_k_norm[0])})
    res = _run(nc, maps)
    QT = _assemble_featmajor([r_["qT_o"] for r_ in res], 8)
    KT = _assemble_featmajor([r_["kT_o"] for r_ in res], 8)
    Vf = _assemble_rows([r_["v_o"] for r_ in res], (16, 65), NPBF)
    QIT = _assemble_featmajor([r_["qiT_o"] for r_ in res], 4)
    KIT = _assemble_featmajor([r_["kiT_o"][None] for r_ in res], 1, 64)[:, 0]
    WI = _assemble_rows([r_["wi_o"] for r_ in res], (8,), f32)
    del res

    nc = _prog("dsamask", lambda: build_dsa_mask(NIT))
    maps = []
    kk = np.arange(512)[None, :]
    for r in range(NCORE):
        b, cp = divmod(r, 4)
        ki2 = np.zeros((128, NKB * 128), NPBF)
        ki2[0:64, :T] = KIT[b]
        ki2[64:128, :T] = KIT[b]
        qic = np.zeros((NIT, 128, 4, 128), NPBF)
        wic = np.zeros((NIT, 128, 8), f32)
        for m in range(NIT):
            i = 4 * m + cp
            if i < NBLK:
                qic[m] = QIT[b][:, :, i * 128:(i + 1) * 128].transpose(1, 0, 2)
                wic[m] = WI[b, i * 128:(i + 1) * 128]
        qq = np.arange(128)[:, None] + cp * 128
        cmask = np.where(kk <= qq, 0.0, -BIGC).astype(f32)
        maps.append({"kiT2": ki2, "qiT": qic, "wi": wic, "cmask": cmask})
    res = _run(nc, maps)
    tot = NBLK * (NBLK + 1) // 2
    MT = [np.zeros((128, tot, 128), NPBF) for _ in range(B)]
    for r in range(NCORE):
        b, cp = divmod(r, 4)
        mt = res[r]["maskT"]
        for m in range(NIT):
            i = 4 * m + cp
            if i < NBLK:
                offm = 2 * m * (m + 1)
                offi = i * (i + 1) // 2
                MT[b][:, offi:offi + i + 1, :] = mt[:, offm:offm + i + 1, :]
    del res, QIT, KIT, WI

    nc = _prog("dsaattn", lambda: build_dsa_attn(NBLK))
    maps = []
    for r in range(NCORE):
        b, hg = divmod(r, 4)
        maps.append({"QT": np.ascontiguousarray(QT[b, 2 * hg:2 * hg + 2].transpose(1, 0, 2)),
                     "KT": np.ascontiguousarray(KT[b, 2 * hg:2 * hg + 2].transpose(1, 0, 2)),
                     "V": np.ascontiguousarray(Vf[b, :, 4 * hg:4 * hg + 4, :]),
                     "MT": MT[b]})
    res = _run(nc, maps)
    o2 = np.zeros((B, T, D), NPBF)
    for r in range(NCORE):
        b, hg = divmod(r, 4)
        o2[b, :, hg * 256:(hg + 1) * 256] = res[r]["O"].reshape(T, 256)
    del res, QT, KT, Vf, MT

    nc = _prog("ffn1", lambda: build_ffnp(True))
    maps = []
    for r in range(NCORE):
        b, c = divmod(r, 4)
        maps.append({"h_in": _shard_rows(h2, b, c), "o_in": _shard_rows(o2, b, c), "wo_in": A(dsa_w_o[0]), "g_in": A(ffn_norm[1]),
                     "wup_in": A(ffn_w_up[1]), "cpar_in": _cpar(A(ffn_conv_w[1]), A(ffn_conv_b[1])), "wdn_in": A(ffn_w_down[1]),
                     "gf_in": A(final_norm)})
    res = _run(nc, maps)
    out = _assemble_rows([r_["h_out"] for r_ in res], (D,), f32)
    return np.ascontiguousarray(out[:, NMETA:NMETA + L])


def build_fused(nblk=NBLK):
    nc = bass.Bass("TRN2", target_bir_lowering=False)
    tt = nblk * 128
    tot = nblk * (nblk + 1) // 2
    def EI(name, shape, dt=F32):
        return nc.dram_tensor(name, list(shape), dt, kind="ExternalInput").ap()
    def IN(name, shape, dt):
        return nc.dram_tensor(name, list(shape), dt, kind="Internal").ap()
    h0 = EI("h0", [tt, D]); cs = EI("cs", [128, nblk, 4, 32]); cmask4 = EI("cmask4", [4, 128, 512])
    da_g = EI("da_g", [D]); w_qkv = EI("w_qkv", [D, 3072]); lamv = EI("lamv", [4, 64]); subg = EI("subg", [128])
    wo0 = EI("wo0", [D, D]); gf0 = EI("gf0", [D]); wup0 = EI("wup0", [D, 2 * FF]); cpar0 = EI("cpar0", [128, 2 * NFC, 4]); wdn0 = EI("wdn0", [FF, D])
    dsa_g = EI("dsa_g", [D]); w_dsa = EI("w_dsa", [D, 3656]); idxg = EI("idxg", [64])
    wo1 = EI("wo1", [D, D]); gf1 = EI("gf1", [D]); wup1 = EI("wup1", [D, 2 * FF]); cpar1 = EI("cpar1", [128, 2 * NFC, 4]); wdn1 = EI("wdn1", [FF, D])
    gfin = EI("gfin", [D])
    out = nc.dram_tensor("out", [tt, D], F32, kind="ExternalOutput").ap()
    qT1 = IN("qT1", [8, 128, tt], BF16); kT1 = IN("kT1", [8, 128, tt], BF16); v1 = IN("v1", [tt, 8, 129], BF16)
    o1 = IN("o1", [tt, D], BF16); h2 = IN("h2", [tt, D], F32)
    qT2 = IN("qT2", [8, 128, tt], BF16); kT2 = IN("kT2", [8, 128, tt], BF16); v2 = IN("v2", [tt, 16, 65], BF16)
    qiT = IN("qiT_s", [4, 128, tt], BF16); kiT = IN("kiT_s", [64, tt], BF16); wi = IN("wi_s", [tt, 8], F32)
    isplit = min(96, nblk)
    nsplit = isplit * (isplit + 1) // 2
    MTa = IN("MTa_s", [128, max(1, nsplit), 128], BF16)
    MTb = IN("MTb_s", [128, max(1, tot - nsplit), 128], BF16)
    MT = lambda i: (MTa, i * (i + 1) // 2) if i < isplit else (MTb, i * (i + 1) // 2 - nsplit)
    o2 = IN("o2", [tt, D], BF16)
    with ExitStack() as ges:
        G = Glob(nc, ges)
        G.io = {"h_in": h0, "g_in": da_g, "w_in": w_qkv, "cs_in": cs, "qT_o": qT1, "kT_o": kT1, "v_o": v1}
        build_proj("A", nblk, glob=G)
        G.io = {"QT": qT1, "KT": kT1, "V": v1.rearrange("t h e -> h t e"), "lamv": lamv, "subg": subg,
                "O": o1.rearrange("t (h e) -> h t e", h=8)}
        build_attn0(8, nblk, 0.2, glob=G)
        G.io = {"h_in": h0, "o_in": o1, "wo_in": wo0, "g_in": gf0, "wup_in": wup0, "cpar_in": cpar0, "wdn_in": wdn0, "h_out": h2}
        build_ffnp(False, nblk, glob=G)
        G.io = {"h_in": h2, "g_in": dsa_g, "w_in": w_dsa, "cs_in": cs, "idxg_in": idxg, "qT_o": qT2, "kT_o": kT2, "v_o": v2,
                "qiT_o": qiT, "kiT_o": kiT, "wi_o": wi}
        build_proj("B", nblk, glob=G)
        G.io = {"kiT": kiT, "qiT": qiT, "wi": wi, "cmask4": cmask4, "MT": MT}
        build_dsa_mask(glob=G, fused_nblk=nblk)
        for hg in range(4):
            G.io = {"QT": qT2[2 * hg:2 * hg + 2].rearrange("c p t -> p c t"), "KT": kT2[2 * hg:2 * hg + 2].rearrange("c p t -> p c t"),
                    "V": v2[:, 4 * hg:4 * hg + 4, :], "MT": MT,
                    "O": o2.rearrange("t (h e) -> t h e", e=64)[:, 4 * hg:4 * hg + 4, :]}
            build_dsa_attn(nblk, glob=G)
        G.io = {"h_in": h2, "o_in": o2, "wo_in": wo1, "g_in": gf1, "wup_in": wup1, "cpar_in": cpar1, "wdn_in": wdn1,
                "gf_in": gfin, "h_out": out}
        build_ffnp(True, nblk, glob=G)
    return nc


def fused_inputs(h0b, params, nblk):
    f32 = np.float32
    A = lambda a: np.ascontiguousarray(np.asarray(a, f32))
    tt = nblk * 128
    inv = (np.float32(10000.0) ** (-(np.arange(0, 64, 2, dtype=np.float32)) / np.float32(64))).astype(np.float32)
    ang = (np.arange(tt, dtype=np.float32)[:, None] * inv[None, :]).astype(np.float32)
    cos = np.cos(ang).astype(np.float32)
    sin = np.sin(ang).astype(np.float32)
    csf = np.stack([cos * np.float32(0.125), sin * np.float32(0.125), cos, sin], axis=1)
    cs = np.ascontiguousarray(csf.reshape(nblk, 128, 4, 32).transpose(1, 0, 2, 3))
    kk = np.arange(512)[None, :]
    cm4 = np.stack([np.where(kk <= (np.arange(128)[:, None] + cp * 128), 0.0, -BIGC).astype(f32) for cp in range(4)])
    p = params
    return {
        "h0": np.ascontiguousarray(h0b), "cs": cs, "cmask4": cm4,
        "da_g": A(p["da_norm"][0]), "w_qkv": A(p["da_w_qkv"][0]),
        "lamv": np.stack([A(p["da_lambda_q1"][0]), A(p["da_lambda_k1"][0]), A(p["da_lambda_q2"][0]), A(p["da_lambda_k2"][0])]),
        "subg": A(p["da_subln"][0]), "wo0": A(p["da_w_o"][0]), "gf0": A(p["ffn_norm"][0]), "wup0": A(p["ffn_w_up"][0]),
        "cpar0": _cpar(A(p["ffn_conv_w"][0]), A(p["ffn_conv_b"][0])), "wdn0": A(p["ffn_w_down"][0]),
        "dsa_g": A(p["dsa_norm"][0]), "w_dsa": A(p["dsa_w_in"][0]), "idxg": A(p["dsa_idx_k_norm"][0]),
        "wo1": A(p["dsa_w_o"][0]), "gf1": A(p["ffn_norm"][1]), "wup1": A(p["ffn_w_up"][1]),
        "cpar1": _cpar(A(p["ffn_conv_w"][1]), A(p["ffn_conv_b"][1])), "wdn1": A(p["ffn_w_down"][1]),
        "gfin": A(p["final_norm"]),
    }


def kernel_fused(**inp):
    f32 = np.float32
    x = np.asarray(inp["x"], f32)
    h0 = np.zeros((B, T, D), f32)
    h0[:, :NMETA] = np.asarray(inp["meta_tokens"], f32)[None]
    h0[:, NMETA:NMETA + L] = x
    nc = _prog("fused", lambda: build_fused(NBLK))
    per_b = [fused_inputs(h0[b], inp, NBLK) for b in range(B)]
    maps = [per_b[r % B] for r in range(NCORE)]
    res = _run(nc, maps)
    out = np.stack([res[b]["out"] for b in range(B)])
    return np.ascontiguousarray(out[:, NMETA:NMETA + L])


def kernel(**inputs):
    return kernel_unfused(**inputs)
```
